# Optimizing a Trainium2 kernel written in Bass

```python
import jax, jax.numpy as jnp
from jax import lax
import numpy as np

D_MODEL = 1024
BATCH = 2
SEQ = 8192
DEPTH = 1

N_META = 16
HEAD_DIM = 64
FOX_HEADS = 8
RWKV_HEADS = 8
FOX_WIDTH = FOX_HEADS * HEAD_DIM
RWKV_WIDTH = RWKV_HEADS * HEAD_DIM
MIX_WIDTH = FOX_WIDTH + RWKV_WIDTH
Q_BLOCK = 128
DECAY_LORA = 64
AAA_LORA = 64
GATE_LORA = 128
FOX_COLS = 3 * FOX_WIDTH + FOX_HEADS
RWKV_COLS = 3 * RWKV_WIDTH + DECAY_LORA + AAA_LORA + GATE_LORA
IN_COLS = FOX_COLS + RWKV_COLS
N_EXPERTS = 32
TOP_K = 4
D_EXPERT = D_MODEL
SWIGLU_LIMIT = 7.0
SWIGLU_ALPHA = 1.702
MOE_BLOCK = 128
DEEPNORM_ALPHA = float((2 * DEPTH) ** 0.25)
DEEPNORM_BETA = float((8 * DEPTH) ** -0.25)
LN_EPS = 1e-5
GN_EPS = 64e-5
RMS_EPS = 1e-6
NEG_BIG = -1e30

kernel_name = "fox_rwkv7_hymba_moe_deepnorm"


def layer_norm(h, g, b):
    hf = h.astype(jnp.float32)
    mu = hf.mean(-1, keepdims=True)
    var = jnp.square(hf - mu).mean(-1, keepdims=True)
    return ((hf - mu) * lax.rsqrt(var + LN_EPS) * g + b).astype(h.dtype)


def fox_attention(q, k, v, logf):
    B, L, H, Dh = q.shape
    pad = Q_BLOCK - N_META
    pw = ((0, 0), (pad, 0), (0, 0), (0, 0))
    q, k, v = jnp.pad(q, pw), jnp.pad(k, pw), jnp.pad(v, pw)
    c = jnp.cumsum(jnp.pad(logf.astype(jnp.float32), ((0, 0), (pad, 0), (0, 0))), axis=1)
    Lp = q.shape[1]
    nb = Lp // Q_BLOCK
    cT = c.transpose(0, 2, 1)
    kpos = jnp.arange(Lp)
    qb = q.reshape(B, nb, Q_BLOCK, H, Dh).transpose(1, 0, 2, 3, 4)
    cb = c.reshape(B, nb, Q_BLOCK, H).transpose(1, 0, 3, 2)
    scale = HEAD_DIM ** -0.5

    def one_block(args):
        q_i, c_i, i = args
        s = jnp.einsum('bqhd,bkhd->bhqk', q_i, k).astype(jnp.float32) * scale
        s = s + c_i[..., None] - cT[:, :, None, :]
        qpos = i * Q_BLOCK + jnp.arange(Q_BLOCK)
        mask = (kpos[None, :] <= qpos[:, None]) & (kpos[None, :] >= pad)
        p = jax.nn.softmax(jnp.where(mask, s, NEG_BIG), axis=-1)
        return jnp.einsum('bhqk,bkhd->bqhd', p.astype(v.dtype), v)

    o = lax.map(one_block, (qb, cb, jnp.arange(nb)))
    return o.transpose(1, 0, 2, 3, 4).reshape(B, Lp, H, Dh)[:, pad:]


def rwkv7_scan(r, w, k, v, kk, a):
    B, L, H, N = r.shape
    xs = tuple(jnp.moveaxis(t.astype(jnp.float32), 1, 0) for t in (r, w, k, v, kk, a))

    def step(S, inp):
        r_t, w_t, k_t, v_t, kk_t, a_t = inp
        sa = jnp.einsum('bhij,bhj->bhi', S, -kk_t)
        S = S * w_t[:, :, None, :] + sa[..., None] * (kk_t * a_t)[:, :, None, :] \
            + v_t[..., None] * k_t[:, :, None, :]
        return S, jnp.einsum('bhij,bhj->bhi', S, r_t)

    S0 = jnp.zeros((B, H, N, N), jnp.float32)
    _, y = lax.scan(step, S0, xs)
    return jnp.moveaxis(y, 0, 1)


def hybrid_mixer(h, w_in, b_fgate, fox_norm_g, rwkv_mu, w0, w2, a0, a2, g2,
                 k_k, k_a, r_k, lnx_g, lnx_b, w_out):
    B, L, _ = h.shape
    z = h @ w_in
    z_fox, z_rwkv = z[..., :FOX_COLS], z[..., FOX_COLS:]

    q, kf, vf, f_logit = jnp.split(z_fox, [FOX_WIDTH, 2 * FOX_WIDTH, 3 * FOX_WIDTH], axis=-1)
    to_heads = lambda t: t.reshape(B, L, -1, HEAD_DIM)
    logf = jax.nn.log_sigmoid((f_logit + b_fgate).astype(jnp.float32))
    o_fox = fox_attention(to_heads(q), to_heads(kf), to_heads(vf), logf).astype(jnp.float32)
    o_fox = o_fox * lax.rsqrt(jnp.mean(jnp.square(o_fox), -1, keepdims=True) + RMS_EPS)
    o_fox = (o_fox.reshape(B, L, FOX_WIDTH) * fox_norm_g).astype(h.dtype)

    z_prev = jnp.pad(z_rwkv, ((0, 0), (1, 0), (0, 0)))[:, :-1]
    zs = z_rwkv + (z_prev - z_rwkv) * rwkv_mu
    r, kr, vr, dw, da, dg = jnp.split(
        zs, [RWKV_WIDTH, 2 * RWKV_WIDTH, 3 * RWKV_WIDTH, 3 * RWKV_WIDTH + DECAY_LORA,
             3 * RWKV_WIDTH + DECAY_LORA + AAA_LORA], axis=-1)
    w_log = -jax.nn.softplus(-(w0 + jnp.tanh(dw) @ w2)) - 0.5
    decay = jnp.exp(-jnp.exp(w_log.astype(jnp.float32)))
    a = jax.nn.sigmoid(a0 + da @ a2)
    g = jax.nn.sigmoid(dg) @ g2
    kk = to_heads(kr * k_k).astype(jnp.float32)
    kk = kk / jnp.maximum(jnp.linalg.norm(kk, axis=-1, keepdims=True), 1e-12)
    kr = kr * (1.0 + (a - 1.0) * k_a)
    rh, kh, vh, ah = to_heads(r), to_heads(kr), to_heads(vr), to_heads(a)
    y = rwkv7_scan(rh, to_heads(decay), kh, vh, kk, ah)
    mu = y.mean(-1, keepdims=True)
    var = jnp.square(y - mu).mean(-1, keepdims=True)
    y = ((y - mu) * lax.rsqrt(var + GN_EPS)).reshape(B, L, RWKV_WIDTH) * lnx_g + lnx_b
    bonus = jnp.sum(rh.astype(jnp.float32) * kh.astype(jnp.float32) * r_k, -1, keepdims=True) \
        * vh.astype(jnp.float32)
    y = ((y + bonus.reshape(B, L, RWKV_WIDTH)) * g).astype(h.dtype)

    return jnp.concatenate([o_fox, y], axis=-1) @ w_out


def moe_ffn(h, w_router, b_router, w_gu, b_gu, w_down, b_down):
    n, D = h.shape
    logits = (h @ w_router + b_router).astype(jnp.float32)
    top_vals, top_idx = lax.top_k(logits, TOP_K)
    gates = jax.nn.softmax(top_vals, axis=-1)
    flat_e = top_idx.reshape(-1)
    flat_tok = jnp.arange(n * TOP_K, dtype=jnp.int32) // TOP_K
    flat_g = gates.reshape(-1)
    order = jnp.argsort(flat_e, stable=True)
    e_sorted = flat_e[order]
    counts = jnp.bincount(flat_e, length=N_EXPERTS)
    start = jnp.cumsum(counts) - counts
    pcounts = (counts + MOE_BLOCK - 1) // MOE_BLOCK * MOE_BLOCK
    pend = jnp.cumsum(pcounts)
    pstart = pend - pcounts
    rank = jnp.arange(n * TOP_K) - start[e_sorted]
    dest = pstart[e_sorted] + rank
    n_blocks = -(-(n * TOP_K) // MOE_BLOCK) + N_EXPERTS
    cap = n_blocks * MOE_BLOCK
    slot_tok = jnp.full((cap,), n, jnp.int32).at[dest].set(flat_tok[order])
    slot_gate = jnp.zeros((cap,), jnp.float32).at[dest].set(flat_g[order])
    block_expert = jnp.minimum(
        jnp.searchsorted(pend, jnp.arange(n_blocks) * MOE_BLOCK, side='right'), N_EXPERTS - 1)
    h_pad = jnp.concatenate([h, jnp.zeros((1, D), h.dtype)], axis=0)
    xb = h_pad[slot_tok].reshape(n_blocks, MOE_BLOCK, D)

    def expert_block(args):
        xblk, e = args
        gu = xblk @ w_gu[e] + b_gu[e]
        gate, up = gu[:, :D_EXPERT], gu[:, D_EXPERT:]
        gate = jnp.minimum(gate, SWIGLU_LIMIT)
        up = jnp.clip(up, -SWIGLU_LIMIT, SWIGLU_LIMIT)
        act = (up + 1.0) * (gate * jax.nn.sigmoid(gate * SWIGLU_ALPHA))
        return act @ w_down[e] + b_down[e]

    yb = lax.map(expert_block, (xb, block_expert)).reshape(cap, D)
    y = jax.ops.segment_sum(yb * slot_gate[:, None].astype(yb.dtype), slot_tok, num_segments=n + 1)
    return y[:n].astype(h.dtype)


def setup_inputs(seed: int = 0) -> dict:
    key = jax.random.key(seed)
    ks = iter(jax.random.split(key, 40))
    nrm = lambda shape, s: jax.random.normal(next(ks), shape, jnp.float32) * s
    uni = lambda shape, lo, hi: jax.random.uniform(next(ks), shape, jnp.float32, lo, hi)
    Ld = DEPTH
    return {
        "x": nrm((BATCH, SEQ, D_MODEL), 1.0),
        "meta": nrm((N_META, D_MODEL), 1.0),
        "ln0_g": 1.0 + nrm((D_MODEL,), 0.02),
        "ln0_b": nrm((D_MODEL,), 0.01),
        "w_in": nrm((Ld, D_MODEL, IN_COLS), D_MODEL ** -0.5),
        "b_fgate": uni((Ld, FOX_HEADS), 1.0, 6.0),
        "fox_norm_g": 1.0 + nrm((Ld, FOX_WIDTH), 0.02),
        "rwkv_mu": uni((Ld, RWKV_COLS), 0.0, 1.0),
        "w0": uni((Ld, RWKV_WIDTH), -6.0, 0.0),
        "w2": nrm((Ld, DECAY_LORA, RWKV_WIDTH), 0.1 * DECAY_LORA ** -0.5),
        "a0": nrm((Ld, RWKV_WIDTH), 0.1),
        "a2": nrm((Ld, AAA_LORA, RWKV_WIDTH), 0.1 * AAA_LORA ** -0.5),
        "g2": nrm((Ld, GATE_LORA, RWKV_WIDTH), GATE_LORA ** -0.5),
        "k_k": 0.85 + nrm((Ld, RWKV_WIDTH), 0.02),
        "k_a": 1.0 + nrm((Ld, RWKV_WIDTH), 0.02),
        "r_k": nrm((Ld, RWKV_HEADS, HEAD_DIM), 0.1),
        "lnx_g": 1.0 + nrm((Ld, RWKV_WIDTH), 0.02),
        "lnx_b": nrm((Ld, RWKV_WIDTH), 0.01),
        "w_out": nrm((Ld, MIX_WIDTH, D_MODEL), MIX_WIDTH ** -0.5 * DEEPNORM_BETA),
        "ln1_g": 1.0 + nrm((Ld, D_MODEL), 0.02),
        "ln1_b": nrm((Ld, D_MODEL), 0.01),
        "w_router": nrm((Ld, D_MODEL, N_EXPERTS), D_MODEL ** -0.5),
        "b_router": nrm((Ld, N_EXPERTS), 0.01),
        "w_gu": nrm((Ld, N_EXPERTS, D_MODEL, 2 * D_EXPERT), D_MODEL ** -0.5),
        "b_gu": nrm((Ld, N_EXPERTS, 2 * D_EXPERT), 0.01),
        "w_down": nrm((Ld, N_EXPERTS, D_EXPERT, D_MODEL), D_EXPERT ** -0.5 * DEEPNORM_BETA),
        "b_down": nrm((Ld, N_EXPERTS, D_MODEL), 0.01),
        "ln2_g": 1.0 + nrm((Ld, D_MODEL), 0.02),
        "ln2_b": nrm((Ld, D_MODEL), 0.01),
    }


def reference(x, meta, ln0_g, ln0_b, w_in, b_fgate, fox_norm_g, rwkv_mu, w0, w2, a0, a2, g2,
              k_k, k_a, r_k, lnx_g, lnx_b, w_out, ln1_g, ln1_b, w_router, b_router,
              w_gu, b_gu, w_down, b_down, ln2_g, ln2_b):
    B = x.shape[0]
    h = jnp.concatenate([jnp.broadcast_to(meta[None].astype(x.dtype), (B, N_META, D_MODEL)), x], axis=1)
    h = layer_norm(h, ln0_g, ln0_b)
    for l in range(DEPTH):
        mix = hybrid_mixer(h, w_in[l], b_fgate[l], fox_norm_g[l], rwkv_mu[l], w0[l], w2[l],
                           a0[l], a2[l], g2[l], k_k[l], k_a[l], r_k[l], lnx_g[l], lnx_b[l], w_out[l])
        h = layer_norm(DEEPNORM_ALPHA * h + mix, ln1_g[l], ln1_b[l])
        Bh, L, D = h.shape
        ff = moe_ffn(h.reshape(Bh * L, D), w_router[l], b_router[l], w_gu[l], b_gu[l],
                     w_down[l], b_down[l]).reshape(Bh, L, D)
        h = layer_norm(DEEPNORM_ALPHA * h + ff, ln2_g[l], ln2_b[l])
    return h[:, N_META:]
```

```python
from contextlib import ExitStack
import numpy as np
import ml_dtypes
import concourse.bass as bass
import concourse.mybir as mybir
from concourse.bass_utils import run_bass_kernel_spmd

F32 = mybir.dt.float32
BF16 = mybir.dt.bfloat16
AF = mybir.ActivationFunctionType
ALU = mybir.AluOpType
AX = mybir.AxisListType

D = 1024
NCORES = 8
LN_EPS = 1e-5
GN_EPS = 64e-5
RMS_EPS = 1e-6
ALPHA = float(2 ** 0.25)
NEXP = 32
SWL = 7.0
SWA = 1.702
NEGPAD = -30000.0

SAME_ENGINE_SYNC = True


class View:
    __slots__ = ("t", "ap")

    def __init__(self, t, ap):
        self.t = t
        self.ap = ap


class Tl:
    __slots__ = ("h", "w", "rs", "name", "excl")

    def __init__(self, h, name="", excl=False):
        self.excl = excl
        self.h = h
        self.w = None
        self.rs = {}
        self.name = name

    def __getitem__(self, idx):
        return View(self, self.h[idx])

    def v(self, ap):
        return View(self, ap)


class Sched:
    def __init__(self, nc, n_dma_sems=48):
        self.nc = nc
        self.eng = {"pe": nc.tensor, "act": nc.scalar, "dve": nc.vector,
                    "pool": nc.gpsimd, "sp": nc.sync}
        self.sem = {k: nc.alloc_semaphore(name="s_" + k) for k in self.eng}
        self.cnt = {k: 0 for k in self.eng}
        self.seen = {k: {} for k in self.eng}
        self.dsems = [nc.alloc_semaphore(name="d%d" % i) for i in range(n_dma_sems)]
        self.dval = [0] * n_dma_sems
        self.dnext = 0
        self.dnext_p = 0
        self.n_wait = 0
        self.n_inst = 0
        self.clk = {k: 0.0 for k in self.eng}
        self.tfin = {}
        self.last_start = 0.0

    def _wait(self, e, dep):
        if dep is None:
            return
        kind, key, val = dep
        if kind == "e":
            if key == e and (e in ("pe", "sp") or not SAME_ENGINE_SYNC):
                return
            sem = self.sem[key]
        else:
            sem = self.dsems[key]
        k = (kind, key)
        if self.seen[e].get(k, 0) >= val:
            return
        self.eng[e].wait_ge(sem, val)
        self.seen[e][k] = val
        self.n_wait += 1

    def _deps(self, e, reads, writes):
        for t in reads:
            self._wait(e, t.w)
            if t.excl:
                for (kd, ky), v in t.rs.items():
                    if ky != e:
                        self._wait(e, (kd, ky, v))
        for t in writes:
            self._wait(e, t.w)
            for (kd, ky), v in t.rs.items():
                self._wait(e, (kd, ky, v))

    def _commit(self, dep, reads, writes):
        k = (dep[0], dep[1])
        for t in reads:
            if t.rs.get(k, 0) < dep[2]:
                t.rs[k] = dep[2]
        for t in writes:
            t.w = dep
            t.rs = {}

    def _est(self, e, reads, writes, cost):
        st = self.clk[e]
        for t in list(reads) + list(writes):
            f = self.tfin.get(id(t))
            if f is not None and f + 0.15 > st:
                st = f + 0.15
        fin = st + cost
        self.clk[e] = fin if e != "sp" else st + 0.1
        for t in list(reads) + list(writes):
            if self.tfin.get(id(t), 0.0) < fin:
                self.tfin[id(t)] = fin
        self.last_start = st

    def do(self, e, fn, reads=(), writes=(), n=128, f32=True):
        self._deps(e, reads, writes)
        ins = fn(self.eng[e])
        self.cnt[e] += 1
        ins.then_inc(self.sem[e], 1)
        self._commit(("e", e, self.cnt[e]), reads, writes)
        self.n_inst += 1
        if e == "pe":
            cost = 0.06 + n * (0.0034 if f32 else 0.00085)
        elif e == "act":
            cost = 0.2 + n * 0.001
        elif e == "dve":
            cost = 0.15 + n * 0.0009
        else:
            cost = 0.2 + n * 0.0021
        self._est(e, reads, writes, cost)
        return ins

    def dma(self, q, out, in_, **kw):
        reads, writes = [in_.t], [out.t]
        self._deps(q, reads, writes)
        nh = len(self.dsems) // 2
        if q == "pool":
            i = nh + self.dnext_p
            self.dnext_p = (self.dnext_p + 1) % (len(self.dsems) - nh)
        else:
            i = self.dnext
            self.dnext = (self.dnext + 1) % nh
        if self.dval[i] > 0:
            self._wait(q, ("d", i, self.dval[i]))
        ins = self.eng[q].dma_start(out=out.ap, in_=in_.ap, **kw)
        self.dval[i] += 16
        ins.then_inc(self.dsems[i], 16)
        self._commit(("d", i, self.dval[i]), reads, writes)
        self.n_inst += 1
        self._est("sp", reads, writes, 2.5)

    def barrier(self):
        for e in self.eng:
            for k in self.eng:
                if k != e and self.cnt[k] > 0:
                    self._wait(e, ("e", k, self.cnt[k]))
            for i, v in enumerate(self.dval):
                if v > 0:
                    self._wait(e, ("d", i, v))

    def wait_all(self, e, tiles):
        for t in tiles:
            self._wait(e, t.w)

    @staticmethod
    def _fs(v):
        n = 1
        for d in v.ap.shape[1:]:
            n *= d
        return n

    def cp(self, e, o, a):
        if e == "act":
            return self.do("act", lambda g: g.copy(out=o.ap, in_=a.ap), [a.t], [o.t], n=self._fs(o))
        return self.do(e, lambda g: g.tensor_copy(out=o.ap, in_=a.ap), [a.t], [o.t], n=self._fs(o))

    def tt(self, e, o, a, b, op):
        return self.do(e, lambda g: g.tensor_tensor(out=o.ap, in0=a.ap, in1=b.ap, op=op),
                       [a.t, b.t], [o.t], n=self._fs(o))

    def ts(self, e, o, a, s1, op0, s2=None, op1=None):
        rd = [a.t]
        s1a, s2a = s1, s2
        if isinstance(s1, View):
            rd.append(s1.t)
            s1a = s1.ap
        if isinstance(s2, View):
            rd.append(s2.t)
            s2a = s2.ap
        if op1 is None:
            return self.do(e, lambda g: g.tensor_scalar(out=o.ap, in0=a.ap, scalar1=s1a, scalar2=None,
                                                        op0=op0), rd, [o.t], n=self._fs(o))
        return self.do(e, lambda g: g.tensor_scalar(out=o.ap, in0=a.ap, scalar1=s1a, scalar2=s2a,
                                                    op0=op0, op1=op1), rd, [o.t], n=self._fs(o))

    def stt(self, e, o, a, s, b, op0, op1):
        rd = [a.t, b.t]
        sa = s
        if isinstance(s, View):
            rd.append(s.t)
            sa = s.ap
        return self.do(e, lambda g: g.scalar_tensor_tensor(out=o.ap, in0=a.ap, scalar=sa, in1=b.ap,
                                                           op0=op0, op1=op1), rd, [o.t], n=self._fs(o))

    def act(self, o, a, func, bias=None, scale=None, accum=None):
        rd = [a.t]
        wr = [o.t]
        kw = {}
        if bias is not None:
            if isinstance(bias, View):
                rd.append(bias.t)
                kw["bias"] = bias.ap
            else:
                kw["bias"] = bias
        if scale is not None:
            if isinstance(scale, View):
                rd.append(scale.t)
                kw["scale"] = scale.ap
            else:
                kw["scale"] = scale
        if accum is not None:
            wr.append(accum.t)
            kw["accum_out"] = accum.ap
        return self.do("act", lambda g: g.activation(out=o.ap, in_=a.ap, func=func, **kw), rd, wr,
                       n=self._fs(o))

    def mm(self, o, l, r, start, stop):
        return self.do("pe", lambda g: g.matmul(o.ap, lhsT=l.ap, rhs=r.ap, start=start, stop=stop),
                       [l.t, r.t], [o.t], n=max(64, self._fs(r)), f32=(r.ap.dtype == F32))

    def tr(self, o, a, ident):
        return self.do("pe", lambda g: g.transpose(out=o.ap, in_=a.ap, identity=ident.ap),
                       [a.t, ident.t], [o.t], n=128, f32=(a.ap.dtype == F32))

    def red(self, e, o, a, op=ALU.add):
        return self.do(e, lambda g: g.tensor_reduce(out=o.ap, in_=a.ap, axis=AX.X, op=op), [a.t], [o.t],
                       n=self._fs(a))


RA = {}
_o = 0
for _n, _s in [("wf0", 1024), ("wf1", 1024), ("mu", 640), ("w0", 128), ("a0", 128), ("kk", 128),
               ("ka", 128), ("rk", 128), ("lg", 128), ("lb", 128), ("fg", 128), ("bf", 2)]:
    RA[_n] = (_o, _o + _s)
    _o += _s
NA = _o
CM = {"ident": 0, "miu": 1, "msu": 2, "msl": 3, "ones": 4}


def build(NTX, stage="full", cut=99):
    NT = NTX + 1
    TPC = NTX // 4
    NTOK = TPC * 128
    nc = bass.Bass("TRN2", target_bir_lowering=False)

    def din(name, shape, dt=F32):
        return Tl(nc.dram_tensor(name, shape, dt, kind="ExternalInput").ap(), name)

    def dout(name, shape, dt=F32):
        return Tl(nc.dram_tensor(name, shape, dt, kind="ExternalOutput").ap(), name)

    xb = din("xb", [NT * 128, D])
    lngb = din("lngb", [128, 6, D])
    rowA_d = din("rowA", [128, NA])
    wfox_d = din("wfox", [D, 384])
    wrw_d = din("wrw", [D, 640])
    w2a2_d = din("w2a2", [128, 128])
    g2_d = din("g2", [128, 128])
    cmat_d = din("cmat", [128, 5, 128])
    ccol_d = din("ccol", [128, 2])
    woutc_d = din("woutc", [256, D])
    rsin = Tl(nc.dram_tensor("rsin", [NTX * 128, D], F32).ap(), "rsin")
    rsout = Tl(nc.dram_tensor("rsout", [NTOK, D], F32).ap(), "rsout")
    if stage != "A":
        xc_d = din("xc", [NTOK, D])
        iota_d = din("iota", [128, 512])
        wrt_d = din("wrt", [D, NEXP])
        brt_d = din("brt", [128, NEXP])
        bgu_d = din("bgu", [128, NEXP * 16])
        bdn_d = din("bdn", [NEXP, D])
        wgu_d = din("wgu", [NEXP, D, 2 * D])
        wdn_d = din("wdn", [NEXP, D, D])
        out_d = dout("out", [NTOK, D])
    gin = Tl(nc.dram_tensor("gin", [NTX * 128, 256], BF16).ap(), "gin")
    gout = Tl(nc.dram_tensor("gout", [NCORES * NTX * 128, 256], BF16).ap(), "gout")
    h1_d = Tl(nc.dram_tensor("h1s", [NTOK, D], F32).ap(), "h1s")
    dbg = None
    if stage != "full":
        dbg = dout("dbg", [NTX * 128, 256], BF16)

    S = Sched(nc)

    with ExitStack() as es0:
        def sb(es, name, shape, dt=F32):
            return Tl(es.enter_context(nc.sbuf_tensor("sb_" + name, shape, dt)), name)

        cm = sb(es0, "cm", [128, 5, 128])
        identb = sb(es0, "identb", [128, 128], BF16)
        ln0 = sb(es0, "ln0", [128, 2, D])
        S.dma("sp", cm[:], cmat_d[:])
        S.dma("sp", ln0[:], lngb[:, 0:2, :])
        S.cp("dve", identb[:], cm[:, 0, :])
        ident = cm[:, 0, :]
        MIU, MSU, MSL, ONES = cm[:, 1, :], cm[:, 2, :], cm[:, 3, :], cm[:, 4, :]
        P = [Tl(es0.enter_context(nc.psum_tensor("pb%d" % i, [128, 512], F32)), "pb%d" % i, excl=True)
             for i in range(7)]
        PTb = Tl(es0.enter_context(nc.psum_tensor("ptb", [128, 1024], BF16)), "ptb", excl=True)

        def layer_norm_stats(es_tmp, src, tagname, st6, mv, rs, nmr, eps, width=D):
            nchunk = (width + 511) // 512
            for c in range(nchunk):
                w0_, w1_ = c * 512, min(width, (c + 1) * 512)
                S.do("dve", lambda g: g.bn_stats(out=st6.h[:, c, :], in_=src.ap[:, w0_:w1_]),
                     [src.t], [st6])
            S.do("dve", lambda g: g.bn_aggr(out=mv.h[:, :], in_=st6.h[:, 0:nchunk, :]), [st6], [mv])
            S.act(rs[:, 0:1], mv[:, 1:2], AF.Ln, bias=eps)
            S.act(rs[:, 0:1], rs[:, 0:1], AF.Exp, scale=-0.5)
            S.ts("dve", nmr[:, 0:1], mv[:, 0:1], rs[:, 0:1], ALU.mult, -1.0, ALU.mult)

        with ExitStack() as esA:
            rowA = sb(esA, "rowA", [128, NA])
            ccol = sb(esA, "ccol", [128, 2])
            wfox = sb(esA, "wfox", [128, 8, 384], BF16)
            w1 = sb(esA, "w1", [128, 8, 640], BF16)
            w2 = sb(esA, "w2", [128, 8, 640], BF16)
            w2a2 = sb(esA, "w2a2", [128, 128], BF16)
            g2 = sb(esA, "g2", [128, 128], BF16)
            S.dma("sp", rowA[:], rowA_d[:])
            S.dma("sp", ccol[:], ccol_d[:])
            S.dma("pool", wfox[:], wfox_d.v(wfox_d.h.rearrange("(k p) n -> p k n", p=128)))
            S.dma("pool", w2a2[:], w2a2_d[:])
            S.dma("pool", g2[:], g2_d[:])

            def ra(name):
                a, b = RA[name]
                return rowA[:, a:b]

            with ExitStack() as esW:
                wrwf = sb(esW, "wrwf", [128, 8, 640])
                tmpw = sb(esW, "tmpw", [128, 640])
                S.dma("sp", wrwf[:], wrw_d.v(wrw_d.h.rearrange("(k p) n -> p k n", p=128)))
                for k in range(8):
                    S.tt("dve", tmpw[:], wrwf[:, k, :], ra("mu"), ALU.mult)
                    S.cp("act", w2[:, k, :], tmpw[:])
                    S.tt("dve", w1[:, k, :], wrwf[:, k, :], tmpw[:], ALU.subtract)
                S.barrier()

            qT_h = esA.enter_context(nc.sbuf_tensor("qT", [128, NT * 128], BF16))
            kT_h = esA.enter_context(nc.sbuf_tensor("kT", [128, NT * 128], BF16))
            V_h = esA.enter_context(nc.sbuf_tensor("Vall", [128, NT, 2, 65], BF16))
            qT = [Tl(qT_h[:, i * 128:(i + 1) * 128], "qT%d" % i) for i in range(NT)]
            kT = [Tl(kT_h[:, i * 128:(i + 1) * 128], "kT%d" % i) for i in range(NT)]
            Vt = [Tl(V_h[:, i, :, :], "V%d" % i) for i in range(NT)]
            Vall = Tl(V_h, "Vall")
            S.do("pool", lambda g: g.memset(V_h[:], 1.0), [], [Vall] + Vt)
            negc = sb(esA, "negc", [128, 2, NT])
            cend = sb(esA, "cend", [128, 2])
            cendAll = sb(esA, "cendAll", [128, 2, NT])
            S.do("pool", lambda g: g.memset(cend.h[:], 0.0), [], [cend])
            Hs = sb(esA, "Hs", [128, 64])
            S.do("pool", lambda g: g.memset(Hs.h[:], 0.0), [], [Hs])
            h0T = [sb(esA, "h0T%d" % j, [128, 8, 128], BF16) for j in range(2)]
            h0P = sb(esA, "h0P", [128, 8, 128], BF16)
            lastc = sb(esA, "lastc", [128, 8, 1], BF16)
            S.do("pool", lambda g: g.memset(lastc.h[:], 0.0), [], [lastc])
            xbuf = [sb(esA, "xt%d" % j, [128, D]) for j in range(2)]
            h0b = sb(esA, "h0b", [128, D], BF16)
            junk = sb(esA, "junk", [128, D])
            st6 = sb(esA, "st6", [128, 2, 6])
            mv = sb(esA, "mv", [128, 2])
            rs = sb(esA, "rs", [128, 1])
            nmr = sb(esA, "nmr", [128, 1])
            fl = sb(esA, "fl", [128, 2])
            ctile = sb(esA, "ctile", [128, 2])
            loT = sb(esA, "loT", [128, 256], BF16)
            rkv = sb(esA, "rkv", [128, 384])
            W = {}
            for nm in ["xw", "lw", "a", "g", "kkr", "sq", "kk", "t1", "kmod", "b", "rk", "bonus",
                       "cumS", "d2", "d4", "E1", "E2", "E3", "E4", "Ke", "Be", "RHS", "nU", "y", "yn"]:
                W[nm] = sb(esA, "w_" + nm, [128, 128])
            TM = sb(esA, "TM", [128, 4, 128])
            TT = sb(esA, "TT", [128, 4, 128])
            AT = [sb(esA, "AT%d" % h, [128, 4, 128]) for h in range(2)]
            Zb = [sb(esA, "Z%d" % j, [128, 2, 128]) for j in range(2)]
            Ztb = [sb(esA, "Zt%d" % j, [128, 2, 128]) for j in range(2)]
            TTi = [sb(esA, "TTi%d" % j, [128, 2, 128]) for j in range(2)]
            sm = sb(esA, "sm", [128, 16])
            gC = sb(esA, "gC", [128, 1])
            biasT = sb(esA, "biasT", [128, NT])
            PT = [sb(esA, "PT%d" % j, [128, 128], BF16) for j in range(3)]
            mask_b = sb(esA, "mask_b", [128, 128], BF16)
            S.cp("dve", mask_b[:], MIU)
            mixt = sb(esA, "mixt", [128, 256], BF16)
            mixT = sb(esA, "mixT", [128, 2, 128], BF16)
            pout = sb(esA, "pout", [128, D])
            woutc = sb(esA, "woutc", [128, 2, D], BF16)
            S.dma("pool", woutc[:], woutc_d.v(woutc_d.h.rearrange("(k p) n -> p k n", p=128)))
            osb = sb(esA, "osb", [128, 65])
            maskA = sb(esA, "maskA", [128, 4, 128])
            S.cp("dve", maskA[:, 0, :], MSU)
            S.cp("dve", maskA[:, 1, :], MIU)
            S.cp("dve", maskA[:, 2, :], MSU)
            S.cp("dve", maskA[:, 3, :], MIU)
            ptc = 0


            rkvS = [rkv, sb(esA, "rkv1", [128, 384]), sb(esA, "rkv2", [128, 384])]
            WS = [{}, {}, {}]
            for nm in ["lw", "a", "g", "kk", "kmod", "b", "bonus"]:
                WS[0][nm] = W[nm]
                WS[1][nm] = sb(esA, "w1_" + nm, [128, 128])
                WS[2][nm] = sb(esA, "w2_" + nm, [128, 128])
            LS = []
            for q_ in range(2):
                L_ = {}
                if q_ == 0:
                    L_.update(TM=TM, TT=TT, AT=AT, Zb=Zb, Ztb=Ztb, TTi=TTi, gC=gC)
                    for nm in ["cumS", "d2", "d4", "E1", "E2", "E3", "E4", "Ke", "Be"]:
                        L_[nm] = W[nm]
                else:
                    L_["TM"] = sb(esA, "TM_1", [128, 4, 128])
                    L_["TT"] = sb(esA, "TT_1", [128, 4, 128])
                    L_["AT"] = [sb(esA, "AT%d_1" % h, [128, 4, 128]) for h in range(2)]
                    L_["Zb"] = [sb(esA, "Z%d_1" % j, [128, 2, 128]) for j in range(2)]
                    L_["Ztb"] = [sb(esA, "Zt%d_1" % j, [128, 2, 128]) for j in range(2)]
                    L_["TTi"] = [sb(esA, "TTi%d_1" % j, [128, 2, 128]) for j in range(2)]
                    L_["gC"] = sb(esA, "gC_1", [128, 1])
                    for nm in ["cumS", "d2", "d4", "E1", "E2", "E3", "E4", "Ke", "Be"]:
                        L_[nm] = sb(esA, "L1_" + nm, [128, 128])
                LS.append(L_)
            smP = sb(esA, "smP", [128, 8])
            smR = sb(esA, "smR", [128, 8])
            smA = [sb(esA, "smA%d" % h, [128, 2]) for h in range(2)]
            st6g = sb(esA, "st6g", [128, 2, 6])
            junkA = sb(esA, "junkA", [128, 64])
            biasTh = [biasT, sb(esA, "biasT1", [128, NT])]
            osbh = [osb, sb(esA, "osb1", [128, 65])]
            ptc_box = [0]
            qbd = [sb(esA, "qbd%d" % j, [128, 256], BF16) for j in range(3)]
            for j in range(3):
                S.do("pool", lambda g: g.memset(qbd[j].h[:], 0.0), [], [qbd[j]])
            zrow = sb(esA, "zrow", [128, 256], BF16)
            S.do("pool", lambda g: g.memset(zrow.h[:], 0.0), [], [zrow])
            PT6 = PT + [sb(esA, "PT%d" % j, [128, 128], BF16) for j in range(3, 6)]

            def g_proj(i):
                cur = h0T[i % 2]
                xt = xbuf[i % 2]
                Wp = WS[i % 3]
                rkv_ = rkvS[i % 3]
                if i + 1 < NT:
                    S.dma("sp", xbuf[(i + 1) % 2][:], xb[(i + 1) * 128:(i + 2) * 128, :])
                layer_norm_stats(None, xt[:], "ln0", st6, mv, rs, nmr, LN_EPS)
                yield
                S.act(xt[:], xt[:], AF.Identity, bias=nmr[:, 0:1], scale=rs[:, 0:1])
                S.tt("dve", xt[:], xt[:], ln0[:, 0, :], ALU.mult)
                S.tt("dve", xt[:], xt[:], ln0[:, 1, :], ALU.add)
                if i == 0:
                    S.ts("dve", xt[:], xt[:], ccol[:, 0:1], ALU.mult)
                S.cp("act", h0b[:], xt[:])
                yield
                for h in range(2):
                    a_, b_ = RA["wf%d" % h]
                    S.do("dve", lambda g: g.scalar_tensor_tensor(
                        out=junk.h[:], in0=xt.h[:], scalar=1.0, in1=rowA.h[:, a_:b_],
                        op0=ALU.mult, op1=ALU.mult, accum_out=fl.h[:, h:h + 1]), [xt, rowA], [junk, fl])
                S.tt("dve", fl[:], fl[:], ra("bf"), ALU.add)
                S.act(fl[:], fl[:], AF.Exp, scale=-1.0)
                S.act(fl[:], fl[:], AF.Ln, bias=1.0)
                if i == 0:
                    S.ts("dve", fl[:], fl[:], ccol[:, 0:1], ALU.mult)
                yield
                S.mm(P[1][:, 384:386], MIU, fl[:], True, True)
                S.mm(P[1][:, 386:388], ONES, fl[:], True, True)
                S.tt("dve", negc[:, :, i], P[1][:, 384:386], cend[:], ALU.add)
                S.tt("dve", cend[:], cend[:], P[1][:, 386:388], ALU.add)
                S.cp("dve", cendAll[:, :, i], cend[:])
                if i == 0:
                    S.ts("dve", negc[:, :, 0], negc[:, :, 0], ccol[:, 1:2], ALU.add)
                yield
                for k in range(8):
                    S.tr(PTb[:, k * 128:(k + 1) * 128], h0b[:, k * 128:(k + 1) * 128], identb[:])
                S.cp("act", cur[:], PTb.v(PTb.h[:].rearrange("p (k t) -> p k t", k=8)))
                S.cp("dve", h0P[:, :, 1:128], cur[:, :, 0:127])
                S.cp("pool", h0P[:, :, 0:1], lastc[:])
                S.cp("pool", lastc[:], cur[:, :, 127:128])
                yield
                for k in range(8):
                    S.mm(P[1][:, 0:128], wfox[:, k, 0:128], cur[:, k, :], k == 0, k == 7)
                for k in range(8):
                    S.mm(P[1][:, 128:256], wfox[:, k, 128:256], cur[:, k, :], k == 0, k == 7)
                yield
                for k in range(8):
                    S.mm(P[1][:, 256:384], cur[:, k, :], wfox[:, k, 256:384], k == 0, k == 7)
                S.cp("act", qT[i][:], P[1][:, 0:128])
                S.cp("act", kT[i][:], P[1][:, 128:256])
                S.cp("dve", qbd[i % 3][0:64, 0:128], P[1][0:64, 0:128])
                S.cp("dve", qbd[i % 3][64:128, 128:256], P[1][64:128, 0:128])
                S.cp("act", Vt[i].v(Vt[i].h[:, :, 0:64]),
                     P[1].v(P[1].h[:, 256:384].rearrange("p (h d) -> p h d", h=2)))
                yield
                for k in range(8):
                    S.mm(P[1][:, 0:384], cur[:, k, :], w1[:, k, 0:384], k == 0, False)
                    S.mm(P[1][:, 0:384], h0P[:, k, :], w2[:, k, 0:384], False, k == 7)
                S.cp("act", rkv_[:], P[1][:, 0:384])
                yield
                for grp in range(2):
                    c0 = 384 + grp * 128
                    for k in range(8):
                        S.mm(P[1][:, grp * 128:(grp + 1) * 128], w1[:, k, c0:c0 + 128], cur[:, k, :],
                             k == 0, False)
                        S.mm(P[1][:, grp * 128:(grp + 1) * 128], w2[:, k, c0:c0 + 128], h0P[:, k, :],
                             False, k == 7)
                    yield
                S.act(W["sq"][0:64, :], P[1][0:64, 0:128], AF.Exp, scale=-2.0)
                S.cp("act", loT[64:128, 0:128], P[1][64:128, 0:128])
                S.act(W["xw"][:], P[1][:, 128:256], AF.Exp, scale=-1.0)
                S.ts("dve", W["sq"][0:64, :], W["sq"][0:64, :], 1.0, ALU.add)
                S.do("dve", lambda g: g.reciprocal(out=W["sq"].h[0:64, :], in_=W["sq"].h[0:64, :]), [W["sq"]], [W["sq"]])
                S.ts("dve", loT[0:64, 0:128], W["sq"][0:64, :], 2.0, ALU.mult, -1.0, ALU.add)
                S.ts("dve", W["xw"][:], W["xw"][:], 1.0, ALU.add)
                S.do("dve", lambda g: g.reciprocal(out=W["xw"].h[:], in_=W["xw"].h[:]), [W["xw"]], [W["xw"]])
                S.cp("act", loT[:, 128:256], W["xw"][:])
                S.mm(P[1][:, 0:128], loT[0:64, 0:128], w2a2[0:64, :], True, True)
                S.mm(P[1][:, 128:256], loT[:, 128:256], g2[:, :], True, True)
                S.mm(P[1][:, 256:384], loT[64:128, 0:128], w2a2[64:128, :], True, True)
                yield
                r_, kraw, v_ = rkv_[:, 0:128], rkv_[:, 128:256], rkv_[:, 256:384]
                S.tt("dve", W["xw"][:], P[1][:, 0:128], ra("w0"), ALU.add)
                S.act(W["xw"][:], W["xw"][:], AF.Exp, scale=-1.0)
                S.ts("dve", W["xw"][:], W["xw"][:], 1.0, ALU.add)
                S.do("dve", lambda g: g.reciprocal(out=Wp["lw"].h[:], in_=W["xw"].h[:]), [W["xw"]], [Wp["lw"]])
                S.ts("dve", Wp["lw"][:], Wp["lw"][:], -0.6065306597126334, ALU.mult)
                S.tt("dve", W["t1"][:], P[1][:, 256:384], ra("a0"), ALU.add)
                S.act(W["t1"][:], W["t1"][:], AF.Exp, scale=-1.0)
                S.ts("dve", W["t1"][:], W["t1"][:], 1.0, ALU.add)
                S.do("dve", lambda g: g.reciprocal(out=Wp["a"].h[:], in_=W["t1"].h[:]), [W["t1"]], [Wp["a"]])
                S.cp("act", Wp["g"][:], P[1][:, 128:256])
                yield
                S.tt("dve", W["kkr"][:], kraw, ra("kk"), ALU.mult)
                S.act(W["sq"][:], W["kkr"][:], AF.Square)
                S.red("dve", smP[:, 0:2], W["sq"].v(W["sq"].h[:].rearrange("p (h n) -> p h n", h=2)))
                S.ts("dve", smP[:, 0:2], smP[:, 0:2], 1e-16, ALU.max)
                S.act(smP[:, 2:4], smP[:, 0:2], AF.Ln)
                S.act(smP[:, 2:4], smP[:, 2:4], AF.Exp, scale=-0.5)
                for h in range(2):
                    S.ts("dve", Wp["kk"][:, h * 64:(h + 1) * 64], W["kkr"][:, h * 64:(h + 1) * 64],
                         smP[:, 2 + h:3 + h], ALU.mult)
                yield
                S.stt("dve", W["t1"][:], Wp["a"][:], -1.0, ra("ka"), ALU.add, ALU.mult)
                S.stt("dve", Wp["kmod"][:], W["t1"][:], 1.0, kraw, ALU.add, ALU.mult)
                S.tt("pool", Wp["b"][:], Wp["kk"][:], Wp["a"][:], ALU.mult)
                S.tt("pool", W["rk"][:], r_, Wp["kmod"][:], ALU.mult)
                S.tt("pool", W["rk"][:], W["rk"][:], ra("rk"), ALU.mult)
                S.red("dve", smP[:, 4:6], W["rk"].v(W["rk"].h[:].rearrange("p (h n) -> p h n", h=2)))
                for h in range(2):
                    S.ts("dve", Wp["bonus"][:, h * 64:(h + 1) * 64], rkv_[:, 256 + h * 64:320 + h * 64],
                         smP[:, 4 + h:5 + h], ALU.mult)
                yield

            def g_local(i):
                Wp = WS[i % 3]
                rkv_ = rkvS[i % 3]
                L = LS[i % 2]
                TM_, TT_, AT_, Zb_, Ztb_, TTi_ = L["TM"], L["TT"], L["AT"], L["Zb"], L["Ztb"], L["TTi"]
                r_ = rkv_[:, 0:128]
                lw = Wp["lw"]
                S.mm(P[5][:, 0:128], MIU, lw[:], True, True)
                S.mm(P[5][:, 128:256], ONES, lw[:], True, True)
                S.mm(P[5][:, 256:257], lw[:], ONES.t.v(cm.h[:, 4, 0:1]), True, True)
                S.cp("dve", L["cumS"][:], P[5][:, 0:128])
                S.tt("dve", L["d4"][:], P[5][:, 128:256], L["cumS"][:], ALU.subtract)
                S.act(L["gC"][:], P[5][:, 256:257], AF.Exp)
                yield
                S.act(L["E1"][:], L["cumS"][:], AF.Exp)
                S.act(L["E3"][:], L["cumS"][:], AF.Exp, scale=-1.0)
                S.tt("dve", L["d2"][:], L["cumS"][:], lw[:], ALU.subtract)
                S.act(L["E2"][:], L["d2"][:], AF.Exp)
                S.act(L["E4"][:], L["d4"][:], AF.Exp)
                yield
                S.tt("pool", TM_[:, 0, :], Wp["kk"][:], L["E2"][:], ALU.mult)
                S.tt("dve", TM_[:, 1, :], r_, L["E1"][:], ALU.mult)
                S.tt("dve", TM_[:, 2, :], Wp["kmod"][:], L["E3"][:], ALU.mult)
                S.tt("dve", TM_[:, 3, :], Wp["b"][:], L["E3"][:], ALU.mult)
                S.tt("pool", L["Ke"][:], Wp["kmod"][:], L["E4"][:], ALU.mult)
                S.tt("pool", L["Be"][:], Wp["b"][:], L["E4"][:], ALU.mult)
                yield
                for j in range(4):
                    S.tr(P[6][:, j * 128:(j + 1) * 128], TM_[:, j, :], ident)
                S.cp("act", TT_[:], P[6].v(P[6].h[:].rearrange("p (j t) -> p j t", j=4)))
                yield
                for h in range(2):
                    hs = slice(h * 64, (h + 1) * 64)
                    S.mm(P[5][:, 0:256], TT_[hs, 2, :], TT_.v(TT_.h[hs, 0:2, :]), True, True)
                    S.mm(P[5][:, 256:512], TT_[hs, 3, :], TT_.v(TT_.h[hs, 0:2, :]), True, True)
                    S.tt("dve", AT_[h][:], P[5].v(P[5].h[:].rearrange("p (j t) -> p j t", j=4)), maskA[:],
                         ALU.mult)
                    S.mm(P[6][:, h * 128:(h + 1) * 128], TT_[hs, 0, :], TT_[hs, 3, :], True, True)
                    yield
                S.tt("dve", Ztb_[0][:], P[6].v(P[6].h[:, 0:256].rearrange("p (h t) -> p h t", h=2)),
                     MSL.t.v(cm.h[:, 3:4, :].to_broadcast([128, 2, 128])), ALU.mult)
                for h in range(2):
                    S.cp("act", Zb_[0][:, h, :], AT_[h][:, 2, :])
                    S.tt("pool", TTi_[0][:, h, :], ident, AT_[h][:, 2, :], ALU.subtract)
                yield
                zc = 0
                for lev in range(6):
                    zn = 1 - zc
                    last = lev == 5
                    for h in range(2):
                        S.mm(P[5][:, 256 + h * 128:384 + h * 128], Zb_[zc][:, h, :], Ztb_[zc][:, h, :], True, True)
                    if not last:
                        for h in range(2):
                            S.mm(P[5][:, h * 128:(h + 1) * 128], Ztb_[zc][:, h, :], Zb_[zc][:, h, :], True, True)
                    S.cp("dve", Ztb_[zn][:], P[5].v(P[5].h[:, 256:512].rearrange("p (h t) -> p h t", h=2)))
                    if not last:
                        S.cp("act", Zb_[zn][:], P[5].v(P[5].h[:, 0:256].rearrange("p (h t) -> p h t", h=2)))
                    yield
                    for h in range(2):
                        S.mm(P[6][:, h * 128:(h + 1) * 128], Ztb_[zn][:, h, :], TTi_[zc][:, h, :], True, True)
                    S.tt("dve", TTi_[zn][:], TTi_[zc][:],
                         P[6].v(P[6].h[:, 0:256].rearrange("p (h t) -> p h t", h=2)), ALU.add)
                    zc = zn
                    yield
                assert zc == 0

            def g_state(i):
                rkv_ = rkvS[i % 3]
                L = LS[i % 2]
                TT_, AT_, Tinv = L["TT"], L["AT"], L["TTi"][0]
                for h in range(2):
                    hs = slice(h * 64, (h + 1) * 64)
                    cs = slice(h * 64, (h + 1) * 64)
                    S.mm(P[2][:, cs], TT_[hs, 0, :], Hs[hs, :], True, False)
                    S.mm(P[2][:, cs], AT_[h][:, 0, :], rkv_[:, 256 + h * 64:320 + h * 64], False, True)
                S.cp("act", W["RHS"][:], P[2][:, 0:128])
                yield
                for h in range(2):
                    cs = slice(h * 64, (h + 1) * 64)
                    S.mm(P[2][:, 128 + h * 64:192 + h * 64], Tinv[:, h, :], W["RHS"][:, cs], True, True)
                S.ts("dve", W["nU"][:], P[2][:, 128:256], -1.0, ALU.mult)
                yield
                for h in range(2):
                    hs = slice(h * 64, (h + 1) * 64)
                    cs = slice(h * 64, (h + 1) * 64)
                    vv = rkv_[:, 256 + h * 64:320 + h * 64]
                    if i > 0:
                        S.mm(P[2][:, 256 + h * 64:320 + h * 64], TT_[hs, 1, :], Hs[hs, :], True, False)
                        S.mm(P[2][:, 256 + h * 64:320 + h * 64], AT_[h][:, 1, :], vv, False, False)
                        S.mm(P[2][:, 256 + h * 64:320 + h * 64], AT_[h][:, 3, :], W["nU"][:, cs], False, True)
                    S.mm(P[2][hs, 384:448], L["Ke"][:, cs], vv, True, False)
                    S.mm(P[2][hs, 384:448], L["Be"][:, cs], W["nU"][:, cs], False, True)
                S.stt("dve", Hs[:], Hs[:], L["gC"][:, 0:1], P[2][:, 384:448], ALU.mult, ALU.add)
                if i > 0:
                    S.cp("act", W["y"][:], P[2][:, 256:384])
                yield

            def g_att(i):
                for h in range(2):
                    S.ts("dve", biasTh[h][:, 0:i + 1], negc[:, h, 0:i + 1], cendAll[:, h, i:i + 1], ALU.subtract)
                S.mm(P[0][:, 0:256], zrow[:, 0:128], zrow[:, 0:256], True, False)
                yield
                qb = qbd[i % 3]
                base = ptc_box[0]
                ptc_box[0] += 2 * (i + 1)
                for j in range(i + 2):
                    if j <= i:
                        sbank = P[3] if (j % 2 == 0) else P[4]
                        S.mm(sbank[:, 0:256], kT[j][:], qb[:], True, True)
                    if j >= 1:
                        jp = j - 1
                        for h in range(2):
                            S.mm(P[0][:, h * 128:h * 128 + 65], PT6[(base + 2 * jp + h) % 6][:], Vt[jp][:, h, :],
                                 False, jp == i and h == 1)
                    if j <= i:
                        for h in range(2):
                            pt = PT6[(base + 2 * j + h) % 6]
                            S.act(pt[:], sbank[:, h * 128:(h + 1) * 128], AF.Exp, bias=biasTh[h][:, j:j + 1],
                                  scale=0.125)
                            if j == i:
                                S.tt("pool", pt[:], pt[:], mask_b[:], ALU.mult)
                    yield
                for h in range(2):
                    ob = osbh[h]
                    sA = smA[h]
                    S.cp("act", ob[:], P[0][:, h * 128:h * 128 + 65])
                    S.do("dve", lambda g: g.reciprocal(out=sA.h[:, 0:1], in_=ob.h[:, 64:65]), [ob], [sA])
                    S.ts("dve", ob[:, 0:64], ob[:, 0:64], sA[:, 0:1], ALU.mult)
                    S.act(junkA[:], ob[:, 0:64], AF.Square, accum=sA[:, 1:2])
                    S.ts("dve", sA[:, 1:2], sA[:, 1:2], 1.0 / 64, ALU.mult, RMS_EPS, ALU.add)
                    S.act(sA[:, 1:2], sA[:, 1:2], AF.Ln)
                    S.act(sA[:, 1:2], sA[:, 1:2], AF.Exp, scale=-0.5)
                    a_, b_ = RA["fg"]
                    S.stt("dve", mixt[:, h * 64:(h + 1) * 64], ob[:, 0:64], sA[:, 1:2],
                          rowA[:, a_ + h * 64:a_ + (h + 1) * 64], ALU.mult, ALU.mult)
                    yield

            def post(i):
                Wp = WS[i % 3]
                for h in range(2):
                    cs = slice(h * 64, (h + 1) * 64)
                    S.do("dve", lambda g: g.bn_stats(out=st6g.h[:, 0, :], in_=W["y"].h[:, cs]), [W["y"]], [st6g])
                    S.do("dve", lambda g: g.bn_aggr(out=smR.h[:, 2 * h:2 + 2 * h], in_=st6g.h[:, 0:1, :]),
                         [st6g], [smR])
                for h in range(2):
                    S.act(smR[:, 6 + h:7 + h], smR[:, 1 + 2 * h:2 + 2 * h], AF.Ln, bias=GN_EPS)
                S.act(smR[:, 6:8], smR[:, 6:8], AF.Exp, scale=-0.5)
                for h in range(2):
                    cs = slice(h * 64, (h + 1) * 64)
                    S.ts("dve", W["yn"][:, cs], W["y"][:, cs], smR[:, 2 * h:1 + 2 * h], ALU.subtract,
                         smR[:, 6 + h:7 + h], ALU.mult)
                S.tt("dve", W["yn"][:], W["yn"][:], ra("lg"), ALU.mult)
                S.tt("pool", W["yn"][:], W["yn"][:], ra("lb"), ALU.add)
                S.tt("pool", W["yn"][:], W["yn"][:], Wp["bonus"][:], ALU.add)
                S.tt("dve", mixt[:, 128:256], W["yn"][:], Wp["g"][:], ALU.mult)
                if stage == "A":
                    S.dma("sp", gin[(i - 1) * 128:i * 128, :], mixt[:])
                for kc in range(2):
                    S.tr(PTb[:, kc * 128:(kc + 1) * 128], mixt[:, kc * 128:(kc + 1) * 128], identb[:])
                S.cp("act", mixT[:], PTb.v(PTb.h[:, 0:256].rearrange("p (k t) -> p k t", k=2)))
                for half in range(2):
                    for kc in range(2):
                        S.mm(P[1][:, 0:512], mixT[:, kc, :], woutc[:, kc, half * 512:(half + 1) * 512],
                             kc == 0, kc == 1)
                    S.cp("act", pout[:, half * 512:(half + 1) * 512], P[1][:, 0:512])
                S.dma("sp", rsin[(i - 1) * 128:i * 128, :], pout[:])

            def interleave(gens):
                act_ = [[g_, 0.0] for g_, _ in gens]
                while act_:
                    it = min(act_, key=lambda z: z[1])
                    try:
                        next(it[0])
                        it[1] = S.last_start
                    except StopIteration:
                        act_.remove(it)

            S.dma("sp", xbuf[0][:], xb[0:128, :])
            for _ in g_proj(0):
                pass
            interleave([(g_proj(1), 1), (g_local(0), 1)])
            for i in range(NT):
                gens = [(g_state(i), 1)]
                if i + 1 < NT:
                    gens.append((g_local(i + 1), 1))
                if i > 0:
                    gens.append((g_att(i), 1))
                if i + 2 < NT:
                    gens.append((g_proj(i + 2), 1))
                interleave(gens)
                if i > 0:
                    post(i)


        if stage == "A":
            stg = sb(es0, "stg", [128, NTX, 256], BF16)
            S.dma("sp", stg[:], gin.v(gin.h.rearrange("(n p) c -> p n c", p=128)))
            S.dma("sp", dbg.v(dbg.h.rearrange("(n p) c -> p n c", p=128)), stg[:])
            S.wait_all("sp", [dbg])
            print("insts", S.n_inst, "waits", S.n_wait)
            return nc
        S.barrier()
        cc = nc.alloc_semaphore(name="cc")
        S._deps("pool", [rsin], [rsout])
        nc.gpsimd.collective_compute("ReduceScatter", ALU.add, replica_groups=[[0, 1, 2, 3], [4, 5, 6, 7]],
                                     ins=[rsin.h.opt()], outs=[rsout.h.opt()]).then_inc(cc)
        for e in S.eng:
            S.eng[e].wait_ge(cc, 1)

        GS = min(512, NTOK)
        NTG = NTOK // GS
        with ExitStack() as esC:
            CAP = min(384, NTOK)
            NSC = CAP // 128
            h1bT = sb(esC, "h1bT", [128, TPC, D], BF16)
            posm = sb(esC, "posm", [128, TPC, NEXP])
            carry = sb(esC, "carry", [128, NEXP])
            S.do("pool", lambda g: g.memset(carry.h[:], 0.0), [], [carry])
            yacc = sb(esC, "yacc", [128, TPC, D])
            Gall = sb(esC, "Gall", [128, TPC, NEXP])
            st6 = sb(esC, "c_st6", [128, 2, 6])
            mv = sb(esC, "c_mv", [128, 2])
            rs = sb(esC, "c_rs", [128, 1])
            nmr = sb(esC, "c_nmr", [128, 1])
            with ExitStack() as esC1:
                lnr = sb(esC1, "lnr", [128, 2, D])
                wrt = sb(esC1, "wrt", [128, 8, NEXP])
                brt = sb(esC1, "brt", [128, NEXP])
                S.dma("sp", lnr[:], lngb[:, 2:4, :])
                S.dma("sp", wrt[:], wrt_d.v(wrt_d.h.rearrange("(k p) n -> p k n", p=128)))
                S.dma("sp", brt[:], brt_d[:])
                xt = sb(esC1, "c_xt", [128, D])
                mm_ = sb(esC1, "c_mm", [128, D])
                h1 = sb(esC1, "c_h1", [128, D])
                h1b = sb(esC1, "c_h1b", [128, D], BF16)
                h1Tf = sb(esC1, "c_h1Tf", [128, 8, 128])
                lg = sb(esC1, "c_lg", [128, NEXP])
                ex = sb(esC1, "c_ex", [128, NEXP])
                msk = sb(esC1, "c_msk", [128, NEXP])
                t8 = sb(esC1, "c_t8", [128, 8])
                sm = sb(esC1, "c_sm", [128, 4])
                for tt in range(TPC):
                    rows = slice(tt * 128, (tt + 1) * 128)
                    S.dma("sp", xt[:], xc_d[rows, :])
                    S.dma("sp", mm_[:], rsout[rows, :])
                    layer_norm_stats(None, xt[:], "ln0c", st6, mv, rs, nmr, LN_EPS)
                    S.act(xt[:], xt[:], AF.Identity, bias=nmr[:, 0:1], scale=rs[:, 0:1])
                    S.tt("dve", xt[:], xt[:], ln0[:, 0, :], ALU.mult)
                    S.tt("pool", xt[:], xt[:], ln0[:, 1, :], ALU.add)
                    S.stt("dve", xt[:], xt[:], ALPHA, mm_[:], ALU.mult, ALU.add)
                    layer_norm_stats(None, xt[:], "ln1", st6, mv, rs, nmr, LN_EPS)
                    S.act(xt[:], xt[:], AF.Identity, bias=nmr[:, 0:1], scale=rs[:, 0:1])
                    S.tt("dve", xt[:], xt[:], lnr[:, 0, :], ALU.mult)
                    S.tt("pool", h1[:], xt[:], lnr[:, 1, :], ALU.add)
                    S.dma("sp", h1_d[rows, :], h1[:])
                    S.cp("act", h1bT[:, tt, :], h1[:])
                    for grp in range(2):
                        for k4 in range(4):
                            k = grp * 4 + k4
                            S.tr(P[grp][:, k4 * 128:(k4 + 1) * 128], h1[:, k * 128:(k + 1) * 128], ident)
                        S.cp("act", h1Tf[:, grp * 4:(grp + 1) * 4, :],
                             P[grp].v(P[grp].h[:].rearrange("p (k t) -> p k t", k=4)))
                    for k in range(8):
                        S.mm(P[2][:, 0:NEXP], h1Tf[:, k, :], wrt[:, k, :], k == 0, k == 7)
                    S.tt("dve", lg[:], P[2][:, 0:NEXP], brt[:], ALU.add)
                    S.do("dve", lambda g: g.max(out=t8.h[:], in_=lg.h[:]), [lg], [t8])
                    S.ts("dve", msk[:], lg[:], t8[:, 3:4], ALU.is_ge)
                    S.ts("dve", sm[:, 0:1], t8[:, 0:1], -1.0, ALU.mult)
                    S.act(ex[:], lg[:], AF.Exp, bias=sm[:, 0:1])
                    S.tt("dve", ex[:], ex[:], msk[:], ALU.mult)
                    S.red("dve", sm[:, 1:2], ex[:])
                    S.do("dve", lambda g: g.reciprocal(out=sm.h[:, 2:3], in_=sm.h[:, 1:2]), [sm], [sm])
                    S.ts("dve", Gall[:, tt, :], ex[:], sm[:, 2:3], ALU.mult)
                    S.mm(P[3][:, 0:NEXP], MIU, msk[:], True, True)
                    S.mm(P[3][:, NEXP:2 * NEXP], ONES, msk[:], True, True)
                    S.tt("dve", lg[:], P[3][:, 0:NEXP], carry[:], ALU.add)
                    S.tt("dve", lg[:], lg[:], msk[:], ALU.mult)
                    S.ts("dve", posm[:, tt, :], lg[:], -1.0, ALU.add)
                    S.tt("dve", carry[:], carry[:], P[3][:, NEXP:2 * NEXP], ALU.add)
                S.barrier()
            with ExitStack() as esE:
                iota = sb(esE, "iota", [128, 512])
                S.dma("sp", iota[:], iota_d[:])
                bgu = sb(esE, "bgu", [128, NEXP * 16])
                S.dma("sp", bgu[:], bgu_d[:])
                bgu1 = sb(esE, "bgu1", [128, NEXP * 16])
                S.ts("dve", bgu1[:], bgu[:], 1.0, ALU.add)
                Sel = sb(esE, "Sel", [128, TPC, CAP], BF16)
                SelT = sb(esE, "SelT", [128, NSC, NTOK], BF16)
                xeT = sb(esE, "xeT", [128, 8, CAP], BF16)
                actT = sb(esE, "actT", [128, 8, CAP], BF16)
                oe = sb(esE, "oe", [128, NSC, D], BF16)
                NPB = 5
                pieces = [sb(esE, "wp%d" % j, [128, 8, 256], BF16) for j in range(NPB)]
                wds = [sb(esE, "wd%d" % j, [128, 8, 512], BF16) for j in range(2)]
                g1 = [sb(esE, "g1_%d" % j, [128, CAP]) for j in range(2)]
                sg = [sb(esE, "sg_%d" % j, [128, CAP]) for j in range(2)]
                u1 = [sb(esE, "u1_%d" % j, [128, CAP]) for j in range(2)]
                S.do("pool", lambda g: g.memset(yacc.h[:], 0.0), [], [yacc])
                pc = 0
                ec = 0
                TG = min(8, TPC)
                for e in range(NEXP):
                    wgv = wgu_d.h[e].rearrange("(k p) n -> p k n", p=128)
                    wdv = wdn_d.h[e].rearrange("(k p) n -> p k n", p=128)
                    for tt in range(TPC):
                        S.ts("dve", Sel[:, tt, :], iota[:, 0:CAP], posm[:, tt, e:e + 1], ALU.is_equal)
                    for k in range(8):
                        pg_ = P[k % 2]
                        for tt in range(TPC):
                            S.mm(pg_[:, 0:CAP], h1bT[:, tt, k * 128:(k + 1) * 128], Sel[:, tt, :],
                                 tt == 0, tt == TPC - 1)
                        S.cp("act" if k % 2 == 0 else "dve", xeT[:, k, :], pg_[:, 0:CAP])
                    for sc in range(NSC):
                        for g0 in range(0, TPC, TG):
                            for q in range(TG):
                                S.tr(PTb[:, q * 128:(q + 1) * 128], Sel[:, g0 + q, sc * 128:(sc + 1) * 128], identb[:])
                            S.cp("act", SelT[:, sc, g0 * 128:(g0 + TG) * 128], PTb[:, 0:TG * 128])
                    for c in range(8):
                        pw = pieces[pc % NPB]
                        pc += 1
                        S.dma("pool", pw[:, :, 0:128], wgu_d.v(wgv[:, :, c * 128:(c + 1) * 128]))
                        S.dma("pool", pw[:, :, 128:256], wgu_d.v(wgv[:, :, D + c * 128:D + (c + 1) * 128]))
                        if c == 3:
                            for half in range(2):
                                S.dma("pool", wds[half][:], wdn_d.v(wdv[:, :, half * 512:(half + 1) * 512]))
                        G1, SG, U1 = g1[ec % 2], sg[ec % 2], u1[ec % 2]
                        pg, pu = P[2 + 2 * (ec % 2)], P[3 + 2 * (ec % 2)]
                        ec += 1
                        for k in range(8):
                            S.mm(pg[:, 0:CAP], pw[:, k, 0:128], xeT[:, k, :], k == 0, k == 7)
                        for k in range(8):
                            S.mm(pu[:, 0:CAP], pw[:, k, 128:256], xeT[:, k, :], k == 0, k == 7)
                        S.ts("dve", G1[:], pg[:, 0:CAP], bgu[:, e * 16 + c:e * 16 + c + 1], ALU.add, SWL, ALU.min)
                        S.act(SG[:], G1[:], AF.Sigmoid, scale=SWA)
                        S.ts("dve", U1[:], pu[:, 0:CAP], bgu1[:, e * 16 + 8 + c:e * 16 + 9 + c], ALU.add,
                             1.0 - SWL, ALU.max)
                        S.tt("dve", G1[:], G1[:], SG[:], ALU.mult)
                        S.stt("dve", actT[:, c, :], U1[:], SWL + 1.0, G1[:], ALU.min, ALU.mult)
                    for sc in range(NSC):
                        for half in range(2):
                            pb = P[half]
                            for k in range(8):
                                S.mm(pb[:, 0:512], actT[:, k, sc * 128:(sc + 1) * 128],
                                     wds[half][:, k, :], k == 0, k == 7)
                            S.cp("act", oe[:, sc, half * 512:(half + 1) * 512], pb[:, 0:512])
                    for tt in range(TPC):
                        for half in range(2):
                            pb = P[6] if half == 0 else P[1]
                            for sc in range(NSC):
                                S.mm(pb[:, 0:512], SelT[:, sc, tt * 128:(tt + 1) * 128],
                                     oe[:, sc, half * 512:(half + 1) * 512], sc == 0, sc == NSC - 1)
                            S.stt("dve", yacc[:, tt, half * 512:(half + 1) * 512], pb[:, 0:512],
                                  Gall[:, tt, e:e + 1], yacc[:, tt, half * 512:(half + 1) * 512], ALU.mult, ALU.add)
                S.barrier()
            with ExitStack() as esF:
                lnr2 = sb(esF, "lnr2", [128, 2, D])
                bdn = sb(esF, "bdn", [NEXP, D])
                S.dma("sp", lnr2[:], lngb[:, 4:6, :])
                S.dma("sp", bdn[:], bdn_d[:])
                GT = sb(esF, "GT", [NEXP, 128])
                h1r = [sb(esF, "h1r%d" % j, [128, D]) for j in range(2)]
                for tt in range(TPC):
                    rows = slice(tt * 128, (tt + 1) * 128)
                    hr = h1r[tt % 2]
                    S.dma("sp", hr[:], h1_d[rows, :])
                    S.tr(P[0][0:NEXP, 0:128], Gall[:, tt, :], ident)
                    S.cp("act", GT[:], P[0][0:NEXP, 0:128])
                    for half in range(2):
                        S.mm(P[1 + half][:, 0:512], GT[:], bdn[:, half * 512:(half + 1) * 512], True, True)
                        S.tt("dve", yacc[:, tt, half * 512:(half + 1) * 512],
                             yacc[:, tt, half * 512:(half + 1) * 512], P[1 + half][:, 0:512], ALU.add)
                    S.stt("dve", hr[:], hr[:], ALPHA, yacc[:, tt, :], ALU.mult, ALU.add)
                    layer_norm_stats(None, hr[:], "ln2", st6, mv, rs, nmr, LN_EPS)
                    S.act(hr[:], hr[:], AF.Identity, bias=nmr[:, 0:1], scale=rs[:, 0:1])
                    S.tt("dve", hr[:], hr[:], lnr2[:, 0, :], ALU.mult)
                    S.tt("pool", hr[:], hr[:], lnr2[:, 1, :], ALU.add)
                    S.dma("sp", out_d[rows, :], hr[:])
                S.wait_all("sp", [out_d])
                S.barrier()
    print("insts", S.n_inst, "waits", S.n_wait)
    return nc


def _bc(v):
    v = np.asarray(v, np.float32).reshape(1, -1)
    return np.ascontiguousarray(np.broadcast_to(v, (128, v.shape[1])))


def prep(inp, NTX):
    f = lambda k: np.asarray(inp[k], np.float32)
    x, meta = f("x"), f("meta")
    w_in = f("w_in")[0]
    R0 = 1544
    p = np.arange(128)[:, None]
    q = np.arange(128)[None, :]
    cmat = np.stack([(p == q), (p <= q), (p < q), (p > q), np.ones((128, 128), bool)], 1).astype(np.float32)
    ccol = np.zeros((128, 2), np.float32)
    ccol[112:, 0] = 1.0
    ccol[:112, 1] = NEGPAD
    lngb = np.stack([_bc(f("ln0_g")), _bc(f("ln0_b")), _bc(f("ln1_g")[0]), _bc(f("ln1_b")[0]),
                     _bc(f("ln2_g")[0]), _bc(f("ln2_b")[0])], 1)
    tile0 = np.zeros((128, D), np.float32)
    tile0[112:] = meta
    w_out = f("w_out")[0]
    bgu = f("b_gu")[0].reshape(NEXP, 16, 128).transpose(2, 0, 1).reshape(128, NEXP * 16)
    shared = {
        "lngb": np.ascontiguousarray(lngb), "cmat": np.ascontiguousarray(cmat), "ccol": ccol,
        "iota": np.ascontiguousarray(np.broadcast_to(np.arange(512, dtype=np.float32)[None, :], (128, 512))),
        "wrt": np.ascontiguousarray(f("w_router")[0]), "brt": _bc(f("b_router")[0]),
        "bgu": np.ascontiguousarray(bgu), "bdn": np.ascontiguousarray(f("b_down")[0]),
        "wgu": np.ascontiguousarray(f("w_gu")[0]), "wdn": np.ascontiguousarray(f("w_down")[0]),
    }
    perm = np.concatenate([np.concatenate([np.arange(r * 128, r * 128 + 128),
                                           np.arange(512 + r * 128, 512 + r * 128 + 128)]) for r in range(4)])
    maps = []
    for c in range(NCORES):
        b, hp = c // 4, c % 4
        cs = slice(hp * 128, hp * 128 + 128)
        hsl = np.arange(hp * 128, hp * 128 + 128)
        fcols = np.concatenate([hsl, 512 + hsl, 1024 + hsl])
        rcols = np.concatenate([R0 + hsl, R0 + 512 + hsl, R0 + 1024 + hsl, R0 + 1536 + np.arange(256)])
        mucols = rcols - R0
        rowA = np.zeros((128, NA), np.float32)

        def put(name, v):
            a, b_ = RA[name]
            rowA[:, a:b_] = _bc(v)
        put("wf0", w_in[:, 1536 + 2 * hp])
        put("wf1", w_in[:, 1536 + 2 * hp + 1])
        put("mu", f("rwkv_mu")[0][mucols])
        put("w0", f("w0")[0][cs]); put("a0", f("a0")[0][cs]); put("kk", f("k_k")[0][cs])
        put("ka", f("k_a")[0][cs]); put("rk", f("r_k")[0].reshape(-1)[cs])
        put("lg", f("lnx_g")[0][cs]); put("lb", f("lnx_b")[0][cs]); put("fg", f("fox_norm_g")[0][cs])
        put("bf", f("b_fgate")[0][2 * hp:2 * hp + 2])
        m = dict(shared)
        m["xb"] = np.ascontiguousarray(np.concatenate([tile0, x[b, :NTX * 128]], 0))
        m["rowA"] = rowA
        m["wfox"] = np.ascontiguousarray(w_in[:, fcols])
        m["wrw"] = np.ascontiguousarray(w_in[:, rcols])
        m["w2a2"] = np.ascontiguousarray(np.concatenate([f("w2")[0][:, cs], f("a2")[0][:, cs]], 0))
        m["g2"] = np.ascontiguousarray(f("g2")[0][:, cs])
        m["woutc"] = np.ascontiguousarray(np.concatenate([w_out[hp * 128:hp * 128 + 128],
                                                          w_out[512 + hp * 128:512 + hp * 128 + 128]], 0))
        TPC = NTX // 4
        m["xc"] = np.ascontiguousarray(x[b, hp * TPC * 128:(hp + 1) * TPC * 128])
        maps.append(m)
    return maps


_NC_CACHE = {}


def kernel(**inputs):
    NTX = 64
    if NTX not in _NC_CACHE:
        _NC_CACHE[NTX] = build(NTX, stage="full")
    nc = _NC_CACHE[NTX]
    maps = prep(inputs, NTX)
    res = run_bass_kernel_spmd(nc, maps, core_ids=list(range(NCORES)))
    TPC = NTX // 4
    out = np.zeros((2, NTX * 128, D), np.float32)
    for c in range(NCORES):
        b, hp = c // 4, c % 4
        out[b, hp * TPC * 128:(hp + 1) * TPC * 128] = np.asarray(res.results[c]["out"], np.float32)
    return out
```

```python
from contextlib import ExitStack
import numpy as np
import ml_dtypes
import concourse.bass as bass
import concourse.mybir as mybir
from concourse.bass_utils import run_bass_kernel_spmd

F32 = mybir.dt.float32
BF16 = mybir.dt.bfloat16
AF = mybir.ActivationFunctionType
ALU = mybir.AluOpType
AX = mybir.AxisListType

D = 1024
NCORES = 8
LN_EPS = 1e-5
GN_EPS = 64e-5
RMS_EPS = 1e-6
ALPHA = float(2 ** 0.25)
NEXP = 32
SWL = 7.0
SWA = 1.702
NEGPAD = -30000.0

SAME_ENGINE_SYNC = True


class View:
    __slots__ = ("t", "ap")

    def __init__(self, t, ap):
        self.t = t
        self.ap = ap


class Tl:
    __slots__ = ("h", "w", "rs", "name", "excl")

    def __init__(self, h, name="", excl=False):
        self.excl = excl
        self.h = h
        self.w = None
        self.rs = {}
        self.name = name

    def __getitem__(self, idx):
        return View(self, self.h[idx])

    def v(self, ap):
        return View(self, ap)


class Sched:
    def __init__(self, nc, n_dma_sems=48):
        self.nc = nc
        self.eng = {"pe": nc.tensor, "act": nc.scalar, "dve": nc.vector,
                    "pool": nc.gpsimd, "sp": nc.sync}
        self.sem = {k: nc.alloc_semaphore(name="s_" + k) for k in self.eng}
        self.cnt = {k: 0 for k in self.eng}
        self.seen = {k: {} for k in self.eng}
        self.dsems = [nc.alloc_semaphore(name="d%d" % i) for i in range(n_dma_sems)]
        self.dval = [0] * n_dma_sems
        self.dnext = 0
        self.dnext_p = 0
        self.n_wait = 0
        self.n_inst = 0
        self.clk = {k: 0.0 for k in self.eng}
        self.tfin = {}
        self.last_start = 0.0

    def _wait(self, e, dep):
        if dep is None:
            return
        kind, key, val = dep
        if kind == "e":
            if key == e and (e in ("pe", "sp") or not SAME_ENGINE_SYNC):
                return
            sem = self.sem[key]
        else:
            sem = self.dsems[key]
        k = (kind, key)
        if self.seen[e].get(k, 0) >= val:
            return
        self.eng[e].wait_ge(sem, val)
        self.seen[e][k] = val
        self.n_wait += 1

    def _deps(self, e, reads, writes):
        for t in reads:
            self._wait(e, t.w)
            if t.excl:
                for (kd, ky), v in t.rs.items():
                    if ky != e:
                        self._wait(e, (kd, ky, v))
        for t in writes:
            self._wait(e, t.w)
            for (kd, ky), v in t.rs.items():
                self._wait(e, (kd, ky, v))

    def _commit(self, dep, reads, writes):
        k = (dep[0], dep[1])
        for t in reads:
            if t.rs.get(k, 0) < dep[2]:
                t.rs[k] = dep[2]
        for t in writes:
            t.w = dep
            t.rs = {}

    def _est(self, e, reads, writes, cost):
        st = self.clk[e]
        for t in list(reads) + list(writes):
            f = self.tfin.get(id(t))
            if f is not None and f + 0.15 > st:
                st = f + 0.15
        fin = st + cost
        self.clk[e] = fin if e != "sp" else st + 0.1
        for t in list(reads) + list(writes):
            if self.tfin.get(id(t), 0.0) < fin:
                self.tfin[id(t)] = fin
        self.last_start = st

    def do(self, e, fn, reads=(), writes=(), n=128, f32=True):
        self._deps(e, reads, writes)
        ins = fn(self.eng[e])
        self.cnt[e] += 1
        ins.then_inc(self.sem[e], 1)
        self._commit(("e", e, self.cnt[e]), reads, writes)
        self.n_inst += 1
        if e == "pe":
            cost = (0.10 + n * 0.00085) if f32 else (0.05 + n * 0.00075)
        elif e == "act":
            cost = 0.2 + n * 0.001
        elif e == "dve":
            cost = 0.15 + n * 0.0009
        else:
            cost = 0.2 + n * 0.0021
        self._est(e, reads, writes, cost)
        return ins

    def dma(self, q, out, in_, **kw):
        reads, writes = [in_.t], [out.t]
        self._deps(q, reads, writes)
        nh = len(self.dsems) // 2
        if q == "pool":
            i = nh + self.dnext_p
            self.dnext_p = (self.dnext_p + 1) % (len(self.dsems) - nh)
        else:
            i = self.dnext
            self.dnext = (self.dnext + 1) % nh
        if self.dval[i] > 0:
            self._wait(q, ("d", i, self.dval[i]))
        ins = self.eng[q].dma_start(out=out.ap, in_=in_.ap, **kw)
        self.dval[i] += 16
        ins.then_inc(self.dsems[i], 16)
        self._commit(("d", i, self.dval[i]), reads, writes)
        self.n_inst += 1
        self._est("sp", reads, writes, 2.5)

    def barrier(self):
        for e in self.eng:
            for k in self.eng:
                if k != e and self.cnt[k] > 0:
                    self._wait(e, ("e", k, self.cnt[k]))
            for i, v in enumerate(self.dval):
                if v > 0:
                    self._wait(e, ("d", i, v))

    def wait_all(self, e, tiles):
        for t in tiles:
            self._wait(e, t.w)

    @staticmethod
    def _fs(v):
        n = 1
        for d in v.ap.shape[1:]:
            n *= d
        return n

    def cp(self, e, o, a):
        if e == "act":
            return self.do("act", lambda g: g.copy(out=o.ap, in_=a.ap), [a.t], [o.t], n=self._fs(o))
        return self.do(e, lambda g: g.tensor_copy(out=o.ap, in_=a.ap), [a.t], [o.t], n=self._fs(o))

    def tt(self, e, o, a, b, op):
        return self.do(e, lambda g: g.tensor_tensor(out=o.ap, in0=a.ap, in1=b.ap, op=op),
                       [a.t, b.t], [o.t], n=self._fs(o))

    def ts(self, e, o, a, s1, op0, s2=None, op1=None):
        rd = [a.t]
        s1a, s2a = s1, s2
        if isinstance(s1, View):
            rd.append(s1.t)
            s1a = s1.ap
        if isinstance(s2, View):
            rd.append(s2.t)
            s2a = s2.ap
        if op1 is None:
            return self.do(e, lambda g: g.tensor_scalar(out=o.ap, in0=a.ap, scalar1=s1a, scalar2=None,
                                                        op0=op0), rd, [o.t], n=self._fs(o))
        return self.do(e, lambda g: g.tensor_scalar(out=o.ap, in0=a.ap, scalar1=s1a, scalar2=s2a,
                                                    op0=op0, op1=op1), rd, [o.t], n=self._fs(o))

    def stt(self, e, o, a, s, b, op0, op1):
        rd = [a.t, b.t]
        sa = s
        if isinstance(s, View):
            rd.append(s.t)
            sa = s.ap
        return self.do(e, lambda g: g.scalar_tensor_tensor(out=o.ap, in0=a.ap, scalar=sa, in1=b.ap,
                                                           op0=op0, op1=op1), rd, [o.t], n=self._fs(o))

    def act(self, o, a, func, bias=None, scale=None, accum=None):
        rd = [a.t]
        wr = [o.t]
        kw = {}
        if bias is not None:
            if isinstance(bias, View):
                rd.append(bias.t)
                kw["bias"] = bias.ap
            else:
                kw["bias"] = bias
        if scale is not None:
            if isinstance(scale, View):
                rd.append(scale.t)
                kw["scale"] = scale.ap
            else:
                kw["scale"] = scale
        if accum is not None:
            wr.append(accum.t)
            kw["accum_out"] = accum.ap
        return self.do("act", lambda g: g.activation(out=o.ap, in_=a.ap, func=func, **kw), rd, wr,
                       n=self._fs(o))

    def mm(self, o, l, r, start, stop):
        return self.do("pe", lambda g: g.matmul(o.ap, lhsT=l.ap, rhs=r.ap, start=start, stop=stop),
                       [l.t, r.t], [o.t], n=max(64, self._fs(r)), f32=(r.ap.dtype == F32))

    def tr(self, o, a, ident):
        return self.do("pe", lambda g: g.transpose(out=o.ap, in_=a.ap, identity=ident.ap),
                       [a.t, ident.t], [o.t], n=128, f32=(a.ap.dtype == F32))

    def red(self, e, o, a, op=ALU.add):
        return self.do(e, lambda g: g.tensor_reduce(out=o.ap, in_=a.ap, axis=AX.X, op=op), [a.t], [o.t],
                       n=self._fs(a))


RA = {}
_o = 0
for _n, _s in [("wf0", 1024), ("wf1", 1024), ("mu", 640), ("w0", 128), ("a0", 128), ("kk", 128),
               ("ka", 128), ("rk", 128), ("lg", 128), ("lb", 128), ("fg", 128), ("bf", 2)]:
    RA[_n] = (_o, _o + _s)
    _o += _s
NA = _o
CM = {"ident": 0, "miu": 1, "msu": 2, "msl": 3, "ones": 4}


def build(NTX, stage="full", cut=99):
    NT = NTX + 1
    TPC = NTX // 4
    NTOK = TPC * 128
    nc = bass.Bass("TRN2", target_bir_lowering=False)

    def din(name, shape, dt=F32):
        return Tl(nc.dram_tensor(name, shape, dt, kind="ExternalInput").ap(), name)

    def dout(name, shape, dt=F32):
        return Tl(nc.dram_tensor(name, shape, dt, kind="ExternalOutput").ap(), name)

    xb = din("xb", [NT * 128, D])
    lngb = din("lngb", [128, 6, D])
    rowA_d = din("rowA", [128, NA])
    wfox_d = din("wfox", [D, 384])
    wrw_d = din("wrw", [D, 640])
    w2a2_d = din("w2a2", [128, 128])
    g2_d = din("g2", [128, 128])
    cmat_d = din("cmat", [128, 5, 128])
    ccol_d = din("ccol", [128, 2])
    woutc_d = din("woutc", [256, D])
    rsin = Tl(nc.dram_tensor("rsin", [NTX * 128, D], F32).ap(), "rsin")
    rsout = Tl(nc.dram_tensor("rsout", [NTOK, D], F32).ap(), "rsout")
    if stage != "A":
        xc_d = din("xc", [NTOK, D])
        iota_d = din("iota", [128, 512])
        wrt_d = din("wrt", [D, NEXP])
        brt_d = din("brt", [128, NEXP])
        bgu_d = din("bgu", [128, NEXP * 16])
        bdn_d = din("bdn", [NEXP, D])
        wgu_d = din("wgu", [NEXP, D, 2 * D])
        wdn_d = din("wdn", [NEXP, D, D])
        out_d = dout("out", [NTOK, D])
    gin = Tl(nc.dram_tensor("gin", [NTX * 128, 256], BF16).ap(), "gin")
    gout = Tl(nc.dram_tensor("gout", [NCORES * NTX * 128, 256], BF16).ap(), "gout")
    h1_d = Tl(nc.dram_tensor("h1s", [NTOK, D], F32).ap(), "h1s")
    dbg = None
    if stage != "full":
        dbg = dout("dbg", [NTX * 128, 256], BF16)

    S = Sched(nc)

    with ExitStack() as es0:
        def sb(es, name, shape, dt=F32):
            return Tl(es.enter_context(nc.sbuf_tensor("sb_" + name, shape, dt)), name)

        cm = sb(es0, "cm", [128, 5, 128])
        identb = sb(es0, "identb", [128, 128], BF16)
        ln0 = sb(es0, "ln0", [128, 2, D])
        S.dma("sp", cm[:], cmat_d[:])
        S.dma("sp", ln0[:], lngb[:, 0:2, :])
        S.cp("dve", identb[:], cm[:, 0, :])
        ident = cm[:, 0, :]
        MIU, MSU, MSL, ONES = cm[:, 1, :], cm[:, 2, :], cm[:, 3, :], cm[:, 4, :]
        P = [Tl(es0.enter_context(nc.psum_tensor("pb%d" % i, [128, 512], F32)), "pb%d" % i, excl=True)
             for i in range(7)]
        PTb = Tl(es0.enter_context(nc.psum_tensor("ptb", [128, 1024], BF16)), "ptb", excl=True)

        def layer_norm_stats(es_tmp, src, tagname, st6, mv, rs, nmr, eps, width=D):
            nchunk = (width + 511) // 512
            for c in range(nchunk):
                w0_, w1_ = c * 512, min(width, (c + 1) * 512)
                S.do("dve", lambda g: g.bn_stats(out=st6.h[:, c, :], in_=src.ap[:, w0_:w1_]),
                     [src.t], [st6])
            S.do("dve", lambda g: g.bn_aggr(out=mv.h[:, :], in_=st6.h[:, 0:nchunk, :]), [st6], [mv])
            S.act(rs[:, 0:1], mv[:, 1:2], AF.Ln, bias=eps)
            S.act(rs[:, 0:1], rs[:, 0:1], AF.Exp, scale=-0.5)
            S.ts("dve", nmr[:, 0:1], mv[:, 0:1], rs[:, 0:1], ALU.mult, -1.0, ALU.mult)

        with ExitStack() as esA:
            rowA = sb(esA, "rowA", [128, NA])
            ccol = sb(esA, "ccol", [128, 2])
            wfox = sb(esA, "wfox", [128, 8, 384], BF16)
            w1 = sb(esA, "w1", [128, 8, 640], BF16)
            w2 = sb(esA, "w2", [128, 8, 640], BF16)
            w2a2 = sb(esA, "w2a2", [128, 128], BF16)
            g2 = sb(esA, "g2", [128, 128], BF16)
            S.dma("sp", rowA[:], rowA_d[:])
            S.dma("sp", ccol[:], ccol_d[:])
            S.dma("pool", wfox[:], wfox_d.v(wfox_d.h.rearrange("(k p) n -> p k n", p=128)))
            S.dma("pool", w2a2[:], w2a2_d[:])
            S.dma("pool", g2[:], g2_d[:])

            def ra(name):
                a, b = RA[name]
                return rowA[:, a:b]

            with ExitStack() as esW:
                wrwf = sb(esW, "wrwf", [128, 8, 640])
                tmpw = sb(esW, "tmpw", [128, 640])
                S.dma("sp", wrwf[:], wrw_d.v(wrw_d.h.rearrange("(k p) n -> p k n", p=128)))
                for k in range(8):
                    S.tt("dve", tmpw[:], wrwf[:, k, :], ra("mu"), ALU.mult)
                    S.cp("act", w2[:, k, :], tmpw[:])
                    S.tt("dve", w1[:, k, :], wrwf[:, k, :], tmpw[:], ALU.subtract)
                S.barrier()

            qT_h = esA.enter_context(nc.sbuf_tensor("qT", [128, NT * 128], BF16))
            kT_h = esA.enter_context(nc.sbuf_tensor("kT", [128, NT * 128], BF16))
            V_h = esA.enter_context(nc.sbuf_tensor("Vall", [128, NT, 2, 65], BF16))
            qT = [Tl(qT_h[:, i * 128:(i + 1) * 128], "qT%d" % i) for i in range(NT)]
            kT = [Tl(kT_h[:, i * 128:(i + 1) * 128], "kT%d" % i) for i in range(NT)]
            Vt = [Tl(V_h[:, i, :, :], "V%d" % i) for i in range(NT)]
            Vall = Tl(V_h, "Vall")
            S.do("pool", lambda g: g.memset(V_h[:], 1.0), [], [Vall] + Vt)
            negc = sb(esA, "negc", [128, 2, NT])
            cend = sb(esA, "cend", [128, 2])
            cendAll = sb(esA, "cendAll", [128, 2, NT])
            S.do("pool", lambda g: g.memset(cend.h[:], 0.0), [], [cend])
            Hs = sb(esA, "Hs", [128, 64])
            S.do("pool", lambda g: g.memset(Hs.h[:], 0.0), [], [Hs])
            h0T = [sb(esA, "h0T%d" % j, [128, 8, 128], BF16) for j in range(2)]
            h0P = sb(esA, "h0P", [128, 8, 128], BF16)
            lastc = sb(esA, "lastc", [128, 8, 1], BF16)
            S.do("pool", lambda g: g.memset(lastc.h[:], 0.0), [], [lastc])
            xbuf = [sb(esA, "xt%d" % j, [128, D]) for j in range(2)]
            h0b = sb(esA, "h0b", [128, D], BF16)
            junk = sb(esA, "junk", [128, D])
            st6 = sb(esA, "st6", [128, 2, 6])
            mv = sb(esA, "mv", [128, 2])
            rs = sb(esA, "rs", [128, 1])
            nmr = sb(esA, "nmr", [128, 1])
            fl = sb(esA, "fl", [128, 2])
            ctile = sb(esA, "ctile", [128, 2])
            loT = sb(esA, "loT", [128, 256], BF16)
            rkv = sb(esA, "rkv", [128, 384])
            W = {}
            for nm in ["xw", "lw", "a", "g", "kkr", "sq", "kk", "t1", "kmod", "b", "rk", "bonus",
                       "cumS", "d2", "d4", "E1", "E2", "E3", "E4", "Ke", "Be", "RHS", "nU", "y", "yn"]:
                W[nm] = sb(esA, "w_" + nm, [128, 128])
            TM = sb(esA, "TM", [128, 4, 128])
            TT = sb(esA, "TT", [128, 4, 128])
            AT = [sb(esA, "AT%d" % h, [128, 4, 128]) for h in range(2)]
            Zb = [sb(esA, "Z%d" % j, [128, 2, 128]) for j in range(2)]
            Ztb = [sb(esA, "Zt%d" % j, [128, 2, 128]) for j in range(2)]
            TTi = [sb(esA, "TTi%d" % j, [128, 2, 128]) for j in range(2)]
            sm = sb(esA, "sm", [128, 16])
            gC = sb(esA, "gC", [128, 1])
            biasT = sb(esA, "biasT", [128, NT])
            PT = [sb(esA, "PT%d" % j, [128, 128], BF16) for j in range(3)]
            mask_b = sb(esA, "mask_b", [128, 128], BF16)
            S.cp("dve", mask_b[:], MIU)
            mixt = sb(esA, "mixt", [128, 256], BF16)
            mixT = sb(esA, "mixT", [128, 2, 128], BF16)
            pout = sb(esA, "pout", [128, D])
            woutc = sb(esA, "woutc", [128, 2, D], BF16)
            S.dma("pool", woutc[:], woutc_d.v(woutc_d.h.rearrange("(k p) n -> p k n", p=128)))
            osb = sb(esA, "osb", [128, 65])
            maskA = sb(esA, "maskA", [128, 4, 128])
            S.cp("dve", maskA[:, 0, :], MSU)
            S.cp("dve", maskA[:, 1, :], MIU)
            S.cp("dve", maskA[:, 2, :], MSU)
            S.cp("dve", maskA[:, 3, :], MIU)
            ptc = 0


            rkvS = [rkv, sb(esA, "rkv1", [128, 384]), sb(esA, "rkv2", [128, 384])]
            WS = [{}, {}, {}]
            for nm in ["lw", "a", "g", "kk", "kmod", "b", "bonus"]:
                WS[0][nm] = W[nm]
                WS[1][nm] = sb(esA, "w1_" + nm, [128, 128])
                WS[2][nm] = sb(esA, "w2_" + nm, [128, 128])
            LS = []
            for q_ in range(2):
                L_ = {}
                if q_ == 0:
                    L_.update(TM=TM, TT=TT, AT=AT, Zb=Zb, Ztb=Ztb, TTi=TTi, gC=gC)
                    for nm in ["cumS", "d2", "d4", "E1", "E2", "E3", "E4", "Ke", "Be"]:
                        L_[nm] = W[nm]
                else:
                    L_["TM"] = sb(esA, "TM_1", [128, 4, 128])
                    L_["TT"] = sb(esA, "TT_1", [128, 4, 128])
                    L_["AT"] = [sb(esA, "AT%d_1" % h, [128, 4, 128]) for h in range(2)]
                    L_["Zb"] = [sb(esA, "Z%d_1" % j, [128, 2, 128]) for j in range(2)]
                    L_["Ztb"] = [sb(esA, "Zt%d_1" % j, [128, 2, 128]) for j in range(2)]
                    L_["TTi"] = [sb(esA, "TTi%d_1" % j, [128, 2, 128]) for j in range(2)]
                    L_["gC"] = sb(esA, "gC_1", [128, 1])
                    for nm in ["cumS", "d2", "d4", "E1", "E2", "E3", "E4", "Ke", "Be"]:
                        L_[nm] = sb(esA, "L1_" + nm, [128, 128])
                LS.append(L_)
            smP = sb(esA, "smP", [128, 8])
            smR = sb(esA, "smR", [128, 8])
            smA = [sb(esA, "smA%d" % h, [128, 2]) for h in range(2)]
            st6g = sb(esA, "st6g", [128, 2, 6])
            junkA = sb(esA, "junkA", [128, 64])
            biasTh = [biasT, sb(esA, "biasT1", [128, NT])]
            osbh = [osb, sb(esA, "osb1", [128, 65])]
            ptc_box = [0]
            qbd = [sb(esA, "qbd%d" % j, [128, 256], BF16) for j in range(3)]
            for j in range(3):
                S.do("pool", lambda g: g.memset(qbd[j].h[:], 0.0), [], [qbd[j]])
            zrow = sb(esA, "zrow", [128, 256], BF16)
            S.do("pool", lambda g: g.memset(zrow.h[:], 0.0), [], [zrow])
            PT6 = PT + [sb(esA, "PT%d" % j, [128, 128], BF16) for j in range(3, 6)]

            def g_proj(i):
                cur = h0T[i % 2]
                xt = xbuf[i % 2]
                Wp = WS[i % 3]
                rkv_ = rkvS[i % 3]
                if i + 1 < NT:
                    S.dma("sp", xbuf[(i + 1) % 2][:], xb[(i + 1) * 128:(i + 2) * 128, :])
                layer_norm_stats(None, xt[:], "ln0", st6, mv, rs, nmr, LN_EPS)
                yield
                S.act(xt[:], xt[:], AF.Identity, bias=nmr[:, 0:1], scale=rs[:, 0:1])
                S.tt("dve", xt[:], xt[:], ln0[:, 0, :], ALU.mult)
                S.tt("dve", xt[:], xt[:], ln0[:, 1, :], ALU.add)
                if i == 0:
                    S.ts("dve", xt[:], xt[:], ccol[:, 0:1], ALU.mult)
                S.cp("act", h0b[:], xt[:])
                yield
                for h in range(2):
                    a_, b_ = RA["wf%d" % h]
                    S.do("dve", lambda g: g.scalar_tensor_tensor(
                        out=junk.h[:], in0=xt.h[:], scalar=1.0, in1=rowA.h[:, a_:b_],
                        op0=ALU.mult, op1=ALU.mult, accum_out=fl.h[:, h:h + 1]), [xt, rowA], [junk, fl])
                S.tt("dve", fl[:], fl[:], ra("bf"), ALU.add)
                S.act(fl[:], fl[:], AF.Exp, scale=-1.0)
                S.act(fl[:], fl[:], AF.Ln, bias=1.0)
                if i == 0:
                    S.ts("dve", fl[:], fl[:], ccol[:, 0:1], ALU.mult)
                yield
                S.mm(P[1][:, 384:386], MIU, fl[:], True, True)
                S.mm(P[1][:, 386:388], ONES, fl[:], True, True)
                S.tt("dve", negc[:, :, i], P[1][:, 384:386], cend[:], ALU.add)
                S.tt("dve", cend[:], cend[:], P[1][:, 386:388], ALU.add)
                S.cp("dve", cendAll[:, :, i], cend[:])
                if i == 0:
                    S.ts("dve", negc[:, :, 0], negc[:, :, 0], ccol[:, 1:2], ALU.add)
                yield
                for k in range(8):
                    S.tr(PTb[:, k * 128:(k + 1) * 128], h0b[:, k * 128:(k + 1) * 128], identb[:])
                S.cp("act", cur[:], PTb.v(PTb.h[:].rearrange("p (k t) -> p k t", k=8)))
                S.cp("dve", h0P[:, :, 1:128], cur[:, :, 0:127])
                S.cp("pool", h0P[:, :, 0:1], lastc[:])
                S.cp("pool", lastc[:], cur[:, :, 127:128])
                yield
                for k in range(8):
                    S.mm(P[1][:, 0:128], wfox[:, k, 0:128], cur[:, k, :], k == 0, k == 7)
                for k in range(8):
                    S.mm(P[1][:, 128:256], wfox[:, k, 128:256], cur[:, k, :], k == 0, k == 7)
                yield
                for k in range(8):
                    S.mm(P[1][:, 256:384], cur[:, k, :], wfox[:, k, 256:384], k == 0, k == 7)
                S.cp("act", qT[i][:], P[1][:, 0:128])
                S.cp("act", kT[i][:], P[1][:, 128:256])
                S.cp("dve", qbd[i % 3][0:64, 0:128], P[1][0:64, 0:128])
                S.cp("dve", qbd[i % 3][64:128, 128:256], P[1][64:128, 0:128])
                S.cp("act", Vt[i].v(Vt[i].h[:, :, 0:64]),
                     P[1].v(P[1].h[:, 256:384].rearrange("p (h d) -> p h d", h=2)))
                yield
                for k in range(8):
                    S.mm(P[1][:, 0:384], cur[:, k, :], w1[:, k, 0:384], k == 0, False)
                    S.mm(P[1][:, 0:384], h0P[:, k, :], w2[:, k, 0:384], False, k == 7)
                S.cp("act", rkv_[:], P[1][:, 0:384])
                yield
                for grp in range(2):
                    c0 = 384 + grp * 128
                    for k in range(8):
                        S.mm(P[1][:, grp * 128:(grp + 1) * 128], w1[:, k, c0:c0 + 128], cur[:, k, :],
                             k == 0, False)
                        S.mm(P[1][:, grp * 128:(grp + 1) * 128], w2[:, k, c0:c0 + 128], h0P[:, k, :],
                             False, k == 7)
                    yield
                S.act(W["sq"][0:64, :], P[1][0:64, 0:128], AF.Exp, scale=-2.0)
                S.cp("act", loT[64:128, 0:128], P[1][64:128, 0:128])
                S.act(W["xw"][:], P[1][:, 128:256], AF.Exp, scale=-1.0)
                S.ts("dve", W["sq"][0:64, :], W["sq"][0:64, :], 1.0, ALU.add)
                S.do("dve", lambda g: g.reciprocal(out=W["sq"].h[0:64, :], in_=W["sq"].h[0:64, :]), [W["sq"]], [W["sq"]])
                S.ts("dve", loT[0:64, 0:128], W["sq"][0:64, :], 2.0, ALU.mult, -1.0, ALU.add)
                S.ts("dve", W["xw"][:], W["xw"][:], 1.0, ALU.add)
                S.do("dve", lambda g: g.reciprocal(out=W["xw"].h[:], in_=W["xw"].h[:]), [W["xw"]], [W["xw"]])
                S.cp("act", loT[:, 128:256], W["xw"][:])
                S.mm(P[1][:, 0:128], loT[0:64, 0:128], w2a2[0:64, :], True, True)
                S.mm(P[1][:, 128:256], loT[:, 128:256], g2[:, :], True, True)
                S.mm(P[1][:, 256:384], loT[64:128, 0:128], w2a2[64:128, :], True, True)
                yield
                r_, kraw, v_ = rkv_[:, 0:128], rkv_[:, 128:256], rkv_[:, 256:384]
                S.tt("dve", W["xw"][:], P[1][:, 0:128], ra("w0"), ALU.add)
                S.act(W["xw"][:], W["xw"][:], AF.Exp, scale=-1.0)
                S.ts("dve", W["xw"][:], W["xw"][:], 1.0, ALU.add)
                S.do("dve", lambda g: g.reciprocal(out=Wp["lw"].h[:], in_=W["xw"].h[:]), [W["xw"]], [Wp["lw"]])
                S.ts("dve", Wp["lw"][:], Wp["lw"][:], -0.6065306597126334, ALU.mult)
                S.tt("dve", W["t1"][:], P[1][:, 256:384], ra("a0"), ALU.add)
                S.act(W["t1"][:], W["t1"][:], AF.Exp, scale=-1.0)
                S.ts("dve", W["t1"][:], W["t1"][:], 1.0, ALU.add)
                S.do("dve", lambda g: g.reciprocal(out=Wp["a"].h[:], in_=W["t1"].h[:]), [W["t1"]], [Wp["a"]])
                S.cp("act", Wp["g"][:], P[1][:, 128:256])
                yield
                S.tt("dve", W["kkr"][:], kraw, ra("kk"), ALU.mult)
                S.act(W["sq"][:], W["kkr"][:], AF.Square)
                S.red("dve", smP[:, 0:2], W["sq"].v(W["sq"].h[:].rearrange("p (h n) -> p h n", h=2)))
                S.ts("dve", smP[:, 0:2], smP[:, 0:2], 1e-16, ALU.max)
                S.act(smP[:, 2:4], smP[:, 0:2], AF.Ln)
                S.act(smP[:, 2:4], smP[:, 2:4], AF.Exp, scale=-0.5)
                for h in range(2):
                    S.ts("dve", Wp["kk"][:, h * 64:(h + 1) * 64], W["kkr"][:, h * 64:(h + 1) * 64],
                         smP[:, 2 + h:3 + h], ALU.mult)
                yield
                S.stt("dve", W["t1"][:], Wp["a"][:], -1.0, ra("ka"), ALU.add, ALU.mult)
                S.stt("dve", Wp["kmod"][:], W["t1"][:], 1.0, kraw, ALU.add, ALU.mult)
                S.tt("pool", Wp["b"][:], Wp["kk"][:], Wp["a"][:], ALU.mult)
                S.tt("pool", W["rk"][:], r_, Wp["kmod"][:], ALU.mult)
                S.tt("pool", W["rk"][:], W["rk"][:], ra("rk"), ALU.mult)
                S.red("dve", smP[:, 4:6], W["rk"].v(W["rk"].h[:].rearrange("p (h n) -> p h n", h=2)))
                for h in range(2):
                    S.ts("dve", Wp["bonus"][:, h * 64:(h + 1) * 64], rkv_[:, 256 + h * 64:320 + h * 64],
                         smP[:, 4 + h:5 + h], ALU.mult)
                yield

            def g_local(i):
                Wp = WS[i % 3]
                rkv_ = rkvS[i % 3]
                L = LS[i % 2]
                TM_, TT_, AT_, Zb_, Ztb_, TTi_ = L["TM"], L["TT"], L["AT"], L["Zb"], L["Ztb"], L["TTi"]
                r_ = rkv_[:, 0:128]
                lw = Wp["lw"]
                S.mm(P[5][:, 0:128], MIU, lw[:], True, True)
                S.mm(P[5][:, 128:256], ONES, lw[:], True, True)
                S.mm(P[5][:, 256:257], lw[:], ONES.t.v(cm.h[:, 4, 0:1]), True, True)
                S.cp("dve", L["cumS"][:], P[5][:, 0:128])
                S.tt("dve", L["d4"][:], P[5][:, 128:256], L["cumS"][:], ALU.subtract)
                S.act(L["gC"][:], P[5][:, 256:257], AF.Exp)
                yield
                S.act(L["E1"][:], L["cumS"][:], AF.Exp)
                S.act(L["E3"][:], L["cumS"][:], AF.Exp, scale=-1.0)
                S.tt("dve", L["d2"][:], L["cumS"][:], lw[:], ALU.subtract)
                S.act(L["E2"][:], L["d2"][:], AF.Exp)
                S.act(L["E4"][:], L["d4"][:], AF.Exp)
                yield
                S.tt("pool", TM_[:, 0, :], Wp["kk"][:], L["E2"][:], ALU.mult)
                S.tt("dve", TM_[:, 1, :], r_, L["E1"][:], ALU.mult)
                S.tt("dve", TM_[:, 2, :], Wp["kmod"][:], L["E3"][:], ALU.mult)
                S.tt("dve", TM_[:, 3, :], Wp["b"][:], L["E3"][:], ALU.mult)
                S.tt("pool", L["Ke"][:], Wp["kmod"][:], L["E4"][:], ALU.mult)
                S.tt("pool", L["Be"][:], Wp["b"][:], L["E4"][:], ALU.mult)
                yield
                for j in range(4):
                    S.tr(P[6][:, j * 128:(j + 1) * 128], TM_[:, j, :], ident)
                S.cp("act", TT_[:], P[6].v(P[6].h[:].rearrange("p (j t) -> p j t", j=4)))
                yield
                for h in range(2):
                    hs = slice(h * 64, (h + 1) * 64)
                    S.mm(P[5][:, 0:256], TT_[hs, 2, :], TT_.v(TT_.h[hs, 0:2, :]), True, True)
                    S.mm(P[5][:, 256:512], TT_[hs, 3, :], TT_.v(TT_.h[hs, 0:2, :]), True, True)
                    S.tt("dve", AT_[h][:], P[5].v(P[5].h[:].rearrange("p (j t) -> p j t", j=4)), maskA[:],
                         ALU.mult)
                    S.mm(P[6][:, h * 128:(h + 1) * 128], TT_[hs, 0, :], TT_[hs, 3, :], True, True)
                    yield
                S.tt("dve", Ztb_[0][:], P[6].v(P[6].h[:, 0:256].rearrange("p (h t) -> p h t", h=2)),
                     MSL.t.v(cm.h[:, 3:4, :].to_broadcast([128, 2, 128])), ALU.mult)
                for h in range(2):
                    S.cp("act", Zb_[0][:, h, :], AT_[h][:, 2, :])
                    S.tt("pool", TTi_[0][:, h, :], ident, AT_[h][:, 2, :], ALU.subtract)
                yield
                zc = 0
                for lev in range(6):
                    zn = 1 - zc
                    last = lev == 5
                    for h in range(2):
                        S.mm(P[5][:, 256 + h * 128:384 + h * 128], Zb_[zc][:, h, :], Ztb_[zc][:, h, :], True, True)
                    if not last:
                        for h in range(2):
                            S.mm(P[5][:, h * 128:(h + 1) * 128], Ztb_[zc][:, h, :], Zb_[zc][:, h, :], True, True)
                    S.cp("dve", Ztb_[zn][:], P[5].v(P[5].h[:, 256:512].rearrange("p (h t) -> p h t", h=2)))
                    if not last:
                        S.cp("act", Zb_[zn][:], P[5].v(P[5].h[:, 0:256].rearrange("p (h t) -> p h t", h=2)))
                    yield
                    for h in range(2):
                        S.mm(P[6][:, h * 128:(h + 1) * 128], Ztb_[zn][:, h, :], TTi_[zc][:, h, :], True, True)
                    S.tt("dve", TTi_[zn][:], TTi_[zc][:],
                         P[6].v(P[6].h[:, 0:256].rearrange("p (h t) -> p h t", h=2)), ALU.add)
                    zc = zn
                    yield
                assert zc == 0

            def g_state(i):
                rkv_ = rkvS[i % 3]
                L = LS[i % 2]
                TT_, AT_, Tinv = L["TT"], L["AT"], L["TTi"][0]
                for h in range(2):
                    hs = slice(h * 64, (h + 1) * 64)
                    cs = slice(h * 64, (h + 1) * 64)
                    S.mm(P[2][:, cs], TT_[hs, 0, :], Hs[hs, :], True, False)
                    S.mm(P[2][:, cs], AT_[h][:, 0, :], rkv_[:, 256 + h * 64:320 + h * 64], False, True)
                S.cp("act", W["RHS"][:], P[2][:, 0:128])
                yield
                for h in range(2):
                    cs = slice(h * 64, (h + 1) * 64)
                    S.mm(P[2][:, 128 + h * 64:192 + h * 64], Tinv[:, h, :], W["RHS"][:, cs], True, True)
                S.ts("dve", W["nU"][:], P[2][:, 128:256], -1.0, ALU.mult)
                yield
                for h in range(2):
                    hs = slice(h * 64, (h + 1) * 64)
                    cs = slice(h * 64, (h + 1) * 64)
                    vv = rkv_[:, 256 + h * 64:320 + h * 64]
                    if i > 0:
                        S.mm(P[2][:, 256 + h * 64:320 + h * 64], TT_[hs, 1, :], Hs[hs, :], True, False)
                        S.mm(P[2][:, 256 + h * 64:320 + h * 64], AT_[h][:, 1, :], vv, False, False)
                        S.mm(P[2][:, 256 + h * 64:320 + h * 64], AT_[h][:, 3, :], W["nU"][:, cs], False, True)
                    S.mm(P[2][hs, 384:448], L["Ke"][:, cs], vv, True, False)
                    S.mm(P[2][hs, 384:448], L["Be"][:, cs], W["nU"][:, cs], False, True)
                S.stt("dve", Hs[:], Hs[:], L["gC"][:, 0:1], P[2][:, 384:448], ALU.mult, ALU.add)
                if i > 0:
                    S.cp("act", W["y"][:], P[2][:, 256:384])
                yield

            def g_att(i):
                for h in range(2):
                    S.ts("dve", biasTh[h][:, 0:i + 1], negc[:, h, 0:i + 1], cendAll[:, h, i:i + 1], ALU.subtract)
                S.mm(P[0][:, 0:256], zrow[:, 0:128], zrow[:, 0:256], True, False)
                yield
                qb = qbd[i % 3]
                base = ptc_box[0]
                ptc_box[0] += 2 * (i + 1)
                for j in range(i + 2):
                    if j <= i:
                        sbank = P[3] if (j % 2 == 0) else P[4]
                        S.mm(sbank[:, 0:256], kT[j][:], qb[:], True, True)
                    if j >= 1:
                        jp = j - 1
                        for h in range(2):
                            S.mm(P[0][:, h * 128:h * 128 + 65], PT6[(base + 2 * jp + h) % 6][:], Vt[jp][:, h, :],
                                 False, jp == i and h == 1)
                    if j <= i:
                        for h in range(2):
                            pt = PT6[(base + 2 * j + h) % 6]
                            S.act(pt[:], sbank[:, h * 128:(h + 1) * 128], AF.Exp, bias=biasTh[h][:, j:j + 1],
                                  scale=0.125)
                            if j == i:
                                S.tt("pool", pt[:], pt[:], mask_b[:], ALU.mult)
                    yield
                for h in range(2):
                    ob = osbh[h]
                    sA = smA[h]
                    S.cp("act", ob[:], P[0][:, h * 128:h * 128 + 65])
                    S.do("dve", lambda g: g.reciprocal(out=sA.h[:, 0:1], in_=ob.h[:, 64:65]), [ob], [sA])
                    S.ts("dve", ob[:, 0:64], ob[:, 0:64], sA[:, 0:1], ALU.mult)
                    S.act(junkA[:], ob[:, 0:64], AF.Square, accum=sA[:, 1:2])
                    S.ts("dve", sA[:, 1:2], sA[:, 1:2], 1.0 / 64, ALU.mult, RMS_EPS, ALU.add)
                    S.act(sA[:, 1:2], sA[:, 1:2], AF.Ln)
                    S.act(sA[:, 1:2], sA[:, 1:2], AF.Exp, scale=-0.5)
                    a_, b_ = RA["fg"]
                    S.stt("dve", mixt[:, h * 64:(h + 1) * 64], ob[:, 0:64], sA[:, 1:2],
                          rowA[:, a_ + h * 64:a_ + (h + 1) * 64], ALU.mult, ALU.mult)
                    yield

            def post(i):
                Wp = WS[i % 3]
                for h in range(2):
                    cs = slice(h * 64, (h + 1) * 64)
                    S.do("dve", lambda g: g.bn_stats(out=st6g.h[:, 0, :], in_=W["y"].h[:, cs]), [W["y"]], [st6g])
                    S.do("dve", lambda g: g.bn_aggr(out=smR.h[:, 2 * h:2 + 2 * h], in_=st6g.h[:, 0:1, :]),
                         [st6g], [smR])
                for h in range(2):
                    S.act(smR[:, 6 + h:7 + h], smR[:, 1 + 2 * h:2 + 2 * h], AF.Ln, bias=GN_EPS)
                S.act(smR[:, 6:8], smR[:, 6:8], AF.Exp, scale=-0.5)
                for h in range(2):
                    cs = slice(h * 64, (h + 1) * 64)
                    S.ts("dve", W["yn"][:, cs], W["y"][:, cs], smR[:, 2 * h:1 + 2 * h], ALU.subtract,
                         smR[:, 6 + h:7 + h], ALU.mult)
                S.tt("dve", W["yn"][:], W["yn"][:], ra("lg"), ALU.mult)
                S.tt("pool", W["yn"][:], W["yn"][:], ra("lb"), ALU.add)
                S.tt("pool", W["yn"][:], W["yn"][:], Wp["bonus"][:], ALU.add)
                S.tt("dve", mixt[:, 128:256], W["yn"][:], Wp["g"][:], ALU.mult)
                if stage == "A":
                    S.dma("sp", gin[(i - 1) * 128:i * 128, :], mixt[:])
                for kc in range(2):
                    S.tr(PTb[:, kc * 128:(kc + 1) * 128], mixt[:, kc * 128:(kc + 1) * 128], identb[:])
                S.cp("act", mixT[:], PTb.v(PTb.h[:, 0:256].rearrange("p (k t) -> p k t", k=2)))
                for half in range(2):
                    for kc in range(2):
                        S.mm(P[1][:, 0:512], mixT[:, kc, :], woutc[:, kc, half * 512:(half + 1) * 512],
                             kc == 0, kc == 1)
                    S.cp("act", pout[:, half * 512:(half + 1) * 512], P[1][:, 0:512])
                S.dma("sp", rsin[(i - 1) * 128:i * 128, :], pout[:])

            def interleave(gens):
                act_ = [[g_, 0.0] for g_, _ in gens]
                while act_:
                    it = min(act_, key=lambda z: z[1])
                    try:
                        next(it[0])
                        it[1] = S.last_start
                    except StopIteration:
                        act_.remove(it)

            S.dma("sp", xbuf[0][:], xb[0:128, :])
            for _ in g_proj(0):
                pass
            interleave([(g_proj(1), 1), (g_local(0), 1)])
            for i in range(NT):
                gens = [(g_state(i), 1)]
                if i + 1 < NT:
                    gens.append((g_local(i + 1), 1))
                if i > 0:
                    gens.append((g_att(i), 1))
                if i + 2 < NT:
                    gens.append((g_proj(i + 2), 1))
                interleave(gens)
                if i > 0:
                    post(i)


        if stage == "A":
            stg = sb(es0, "stg", [128, NTX, 256], BF16)
            S.dma("sp", stg[:], gin.v(gin.h.rearrange("(n p) c -> p n c", p=128)))
            S.dma("sp", dbg.v(dbg.h.rearrange("(n p) c -> p n c", p=128)), stg[:])
            S.wait_all("sp", [dbg])
            print("insts", S.n_inst, "waits", S.n_wait)
            return nc
        S.barrier()
        cc = nc.alloc_semaphore(name="cc")
        S._deps("pool", [rsin], [rsout])
        nc.gpsimd.collective_compute("ReduceScatter", ALU.add, replica_groups=[[0, 1, 2, 3], [4, 5, 6, 7]],
                                     ins=[rsin.h.opt()], outs=[rsout.h.opt()]).then_inc(cc)
        for e in S.eng:
            S.eng[e].wait_ge(cc, 1)

        GS = min(512, NTOK)
        NTG = NTOK // GS
        with ExitStack() as esC:
            CAP = min(384, NTOK)
            NSC = CAP // 128
            h1bT = sb(esC, "h1bT", [128, TPC, D], BF16)
            posm = sb(esC, "posm", [128, TPC, NEXP])
            carry = sb(esC, "carry", [128, NEXP])
            S.do("pool", lambda g: g.memset(carry.h[:], 0.0), [], [carry])
            yacc = sb(esC, "yacc", [128, TPC, D])
            Gall = sb(esC, "Gall", [128, TPC, NEXP])
            st6 = sb(esC, "c_st6", [128, 2, 6])
            mv = sb(esC, "c_mv", [128, 2])
            rs = sb(esC, "c_rs", [128, 1])
            nmr = sb(esC, "c_nmr", [128, 1])
            with ExitStack() as esC1:
                lnr = sb(esC1, "lnr", [128, 2, D])
                wrt = sb(esC1, "wrt", [128, 8, NEXP])
                brt = sb(esC1, "brt", [128, NEXP])
                S.dma("sp", lnr[:], lngb[:, 2:4, :])
                S.dma("sp", wrt[:], wrt_d.v(wrt_d.h.rearrange("(k p) n -> p k n", p=128)))
                S.dma("sp", brt[:], brt_d[:])
                xt = sb(esC1, "c_xt", [128, D])
                mm_ = sb(esC1, "c_mm", [128, D])
                h1 = sb(esC1, "c_h1", [128, D])
                h1b = sb(esC1, "c_h1b", [128, D], BF16)
                h1Tf = sb(esC1, "c_h1Tf", [128, 8, 128])
                lg = sb(esC1, "c_lg", [128, NEXP])
                ex = sb(esC1, "c_ex", [128, NEXP])
                msk = sb(esC1, "c_msk", [128, NEXP])
                t8 = sb(esC1, "c_t8", [128, 8])
                sm = sb(esC1, "c_sm", [128, 4])
                for tt in range(TPC):
                    rows = slice(tt * 128, (tt + 1) * 128)
                    S.dma("sp", xt[:], xc_d[rows, :])
                    S.dma("sp", mm_[:], rsout[rows, :])
                    layer_norm_stats(None, xt[:], "ln0c", st6, mv, rs, nmr, LN_EPS)
                    S.act(xt[:], xt[:], AF.Identity, bias=nmr[:, 0:1], scale=rs[:, 0:1])
                    S.tt("dve", xt[:], xt[:], ln0[:, 0, :], ALU.mult)
                    S.tt("pool", xt[:], xt[:], ln0[:, 1, :], ALU.add)
                    S.stt("dve", xt[:], xt[:], ALPHA, mm_[:], ALU.mult, ALU.add)
                    layer_norm_stats(None, xt[:], "ln1", st6, mv, rs, nmr, LN_EPS)
                    S.act(xt[:], xt[:], AF.Identity, bias=nmr[:, 0:1], scale=rs[:, 0:1])
                    S.tt("dve", xt[:], xt[:], lnr[:, 0, :], ALU.mult)
                    S.tt("pool", h1[:], xt[:], lnr[:, 1, :], ALU.add)
                    S.dma("sp", h1_d[rows, :], h1[:])
                    S.cp("act", h1bT[:, tt, :], h1[:])
                    for grp in range(2):
                        for k4 in range(4):
                            k = grp * 4 + k4
                            S.tr(P[grp][:, k4 * 128:(k4 + 1) * 128], h1[:, k * 128:(k + 1) * 128], ident)
                        S.cp("act", h1Tf[:, grp * 4:(grp + 1) * 4, :],
                             P[grp].v(P[grp].h[:].rearrange("p (k t) -> p k t", k=4)))
                    for k in range(8):
                        S.mm(P[2][:, 0:NEXP], h1Tf[:, k, :], wrt[:, k, :], k == 0, k == 7)
                    S.tt("dve", lg[:], P[2][:, 0:NEXP], brt[:], ALU.add)
                    S.do("dve", lambda g: g.max(out=t8.h[:], in_=lg.h[:]), [lg], [t8])
                    S.ts("dve", msk[:], lg[:], t8[:, 3:4], ALU.is_ge)
                    S.ts("dve", sm[:, 0:1], t8[:, 0:1], -1.0, ALU.mult)
                    S.act(ex[:], lg[:], AF.Exp, bias=sm[:, 0:1])
                    S.tt("dve", ex[:], ex[:], msk[:], ALU.mult)
                    S.red("dve", sm[:, 1:2], ex[:])
                    S.do("dve", lambda g: g.reciprocal(out=sm.h[:, 2:3], in_=sm.h[:, 1:2]), [sm], [sm])
                    S.ts("dve", Gall[:, tt, :], ex[:], sm[:, 2:3], ALU.mult)
                    S.mm(P[3][:, 0:NEXP], MIU, msk[:], True, True)
                    S.mm(P[3][:, NEXP:2 * NEXP], ONES, msk[:], True, True)
                    S.tt("dve", lg[:], P[3][:, 0:NEXP], carry[:], ALU.add)
                    S.tt("dve", lg[:], lg[:], msk[:], ALU.mult)
                    S.ts("dve", posm[:, tt, :], lg[:], -1.0, ALU.add)
                    S.tt("dve", carry[:], carry[:], P[3][:, NEXP:2 * NEXP], ALU.add)
                S.barrier()
            with ExitStack() as esE:
                iota = sb(esE, "iota", [128, 512])
                S.dma("sp", iota[:], iota_d[:])
                bgu = sb(esE, "bgu", [128, NEXP * 16])
                S.dma("sp", bgu[:], bgu_d[:])
                bgu1 = sb(esE, "bgu1", [128, NEXP * 16])
                S.ts("dve", bgu1[:], bgu[:], 1.0, ALU.add)
                Sel = sb(esE, "Sel", [128, TPC, CAP], BF16)
                SelT = sb(esE, "SelT", [128, NSC, NTOK], BF16)
                xeT = sb(esE, "xeT", [128, 8, CAP], BF16)
                actT = sb(esE, "actT", [128, 8, CAP], BF16)
                oe = sb(esE, "oe", [128, NSC, D], BF16)
                NPB = 5
                pieces = [sb(esE, "wp%d" % j, [128, 8, 256], BF16) for j in range(NPB)]
                wds = [sb(esE, "wd%d" % j, [128, 8, 512], BF16) for j in range(2)]
                g1 = [sb(esE, "g1_%d" % j, [128, CAP]) for j in range(2)]
                sg = [sb(esE, "sg_%d" % j, [128, CAP]) for j in range(2)]
                u1 = [sb(esE, "u1_%d" % j, [128, CAP]) for j in range(2)]
                S.do("pool", lambda g: g.memset(yacc.h[:], 0.0), [], [yacc])
                pc = 0
                ec = 0
                TG = min(8, TPC)
                for e in range(NEXP):
                    wgv = wgu_d.h[e].rearrange("(k p) n -> p k n", p=128)
                    wdv = wdn_d.h[e].rearrange("(k p) n -> p k n", p=128)
                    for tt in range(TPC):
                        S.ts("dve", Sel[:, tt, :], iota[:, 0:CAP], posm[:, tt, e:e + 1], ALU.is_equal)
                    for k in range(8):
                        pg_ = P[k % 2]
                        for tt in range(TPC):
                            S.mm(pg_[:, 0:CAP], h1bT[:, tt, k * 128:(k + 1) * 128], Sel[:, tt, :],
                                 tt == 0, tt == TPC - 1)
                        S.cp("act" if k % 2 == 0 else "dve", xeT[:, k, :], pg_[:, 0:CAP])
                    for sc in range(NSC):
                        for g0 in range(0, TPC, TG):
                            for q in range(TG):
                                S.tr(PTb[:, q * 128:(q + 1) * 128], Sel[:, g0 + q, sc * 128:(sc + 1) * 128], identb[:])
                            S.cp("act", SelT[:, sc, g0 * 128:(g0 + TG) * 128], PTb[:, 0:TG * 128])
                    for c in range(8):
                        pw = pieces[pc % NPB]
                        pc += 1
                        S.dma("pool", pw[:, :, 0:128], wgu_d.v(wgv[:, :, c * 128:(c + 1) * 128]))
                        S.dma("pool", pw[:, :, 128:256], wgu_d.v(wgv[:, :, D + c * 128:D + (c + 1) * 128]))
                        if c == 3:
                            for half in range(2):
                                S.dma("pool", wds[half][:], wdn_d.v(wdv[:, :, half * 512:(half + 1) * 512]))
                        G1, SG, U1 = g1[ec % 2], sg[ec % 2], u1[ec % 2]
                        pg, pu = P[2 + 2 * (ec % 2)], P[3 + 2 * (ec % 2)]
                        ec += 1
                        for k in range(8):
                            S.mm(pg[:, 0:CAP], pw[:, k, 0:128], xeT[:, k, :], k == 0, k == 7)
                        for k in range(8):
                            S.mm(pu[:, 0:CAP], pw[:, k, 128:256], xeT[:, k, :], k == 0, k == 7)
                        S.ts("dve", G1[:], pg[:, 0:CAP], bgu[:, e * 16 + c:e * 16 + c + 1], ALU.add, SWL, ALU.min)
                        S.act(SG[:], G1[:], AF.Sigmoid, scale=SWA)
                        S.ts("dve", U1[:], pu[:, 0:CAP], bgu1[:, e * 16 + 8 + c:e * 16 + 9 + c], ALU.add,
                             1.0 - SWL, ALU.max)
                        S.tt("dve", G1[:], G1[:], SG[:], ALU.mult)
                        S.stt("dve", actT[:, c, :], U1[:], SWL + 1.0, G1[:], ALU.min, ALU.mult)
                    for sc in range(NSC):
                        for half in range(2):
                            pb = P[half]
                            for k in range(8):
                                S.mm(pb[:, 0:512], actT[:, k, sc * 128:(sc + 1) * 128],
                                     wds[half][:, k, :], k == 0, k == 7)
                            S.cp("act", oe[:, sc, half * 512:(half + 1) * 512], pb[:, 0:512])
                    for tt in range(TPC):
                        for half in range(2):
                            pb = P[6] if half == 0 else P[1]
                            for sc in range(NSC):
                                S.mm(pb[:, 0:512], SelT[:, sc, tt * 128:(tt + 1) * 128],
                                     oe[:, sc, half * 512:(half + 1) * 512], sc == 0, sc == NSC - 1)
                            S.stt("dve", yacc[:, tt, half * 512:(half + 1) * 512], pb[:, 0:512],
                                  Gall[:, tt, e:e + 1], yacc[:, tt, half * 512:(half + 1) * 512], ALU.mult, ALU.add)
                S.barrier()
            with ExitStack() as esF:
                lnr2 = sb(esF, "lnr2", [128, 2, D])
                bdn = sb(esF, "bdn", [NEXP, D])
                S.dma("sp", lnr2[:], lngb[:, 4:6, :])
                S.dma("sp", bdn[:], bdn_d[:])
                GT = sb(esF, "GT", [NEXP, 128])
                h1r = [sb(esF, "h1r%d" % j, [128, D]) for j in range(2)]
                for tt in range(TPC):
                    rows = slice(tt * 128, (tt + 1) * 128)
                    hr = h1r[tt % 2]
                    S.dma("sp", hr[:], h1_d[rows, :])
                    S.tr(P[0][0:NEXP, 0:128], Gall[:, tt, :], ident)
                    S.cp("act", GT[:], P[0][0:NEXP, 0:128])
                    for half in range(2):
                        S.mm(P[1 + half][:, 0:512], GT[:], bdn[:, half * 512:(half + 1) * 512], True, True)
                        S.tt("dve", yacc[:, tt, half * 512:(half + 1) * 512],
                             yacc[:, tt, half * 512:(half + 1) * 512], P[1 + half][:, 0:512], ALU.add)
                    S.stt("dve", hr[:], hr[:], ALPHA, yacc[:, tt, :], ALU.mult, ALU.add)
                    layer_norm_stats(None, hr[:], "ln2", st6, mv, rs, nmr, LN_EPS)
                    S.act(hr[:], hr[:], AF.Identity, bias=nmr[:, 0:1], scale=rs[:, 0:1])
                    S.tt("dve", hr[:], hr[:], lnr2[:, 0, :], ALU.mult)
                    S.tt("pool", hr[:], hr[:], lnr2[:, 1, :], ALU.add)
                    S.dma("sp", out_d[rows, :], hr[:])
                S.wait_all("sp", [out_d])
                S.barrier()
    print("insts", S.n_inst, "waits", S.n_wait)
    return nc


def _bc(v):
    v = np.asarray(v, np.float32).reshape(1, -1)
    return np.ascontiguousarray(np.broadcast_to(v, (128, v.shape[1])))


def prep(inp, NTX):
    f = lambda k: np.asarray(inp[k], np.float32)
    x, meta = f("x"), f("meta")
    w_in = f("w_in")[0]
    R0 = 1544
    p = np.arange(128)[:, None]
    q = np.arange(128)[None, :]
    cmat = np.stack([(p == q), (p <= q), (p < q), (p > q), np.ones((128, 128), bool)], 1).astype(np.float32)
    ccol = np.zeros((128, 2), np.float32)
    ccol[112:, 0] = 1.0
    ccol[:112, 1] = NEGPAD
    lngb = np.stack([_bc(f("ln0_g")), _bc(f("ln0_b")), _bc(f("ln1_g")[0]), _bc(f("ln1_b")[0]),
                     _bc(f("ln2_g")[0]), _bc(f("ln2_b")[0])], 1)
    tile0 = np.zeros((128, D), np.float32)
    tile0[112:] = meta
    w_out = f("w_out")[0]
    bgu = f("b_gu")[0].reshape(NEXP, 16, 128).transpose(2, 0, 1).reshape(128, NEXP * 16)
    shared = {
        "lngb": np.ascontiguousarray(lngb), "cmat": np.ascontiguousarray(cmat), "ccol": ccol,
        "iota": np.ascontiguousarray(np.broadcast_to(np.arange(512, dtype=np.float32)[None, :], (128, 512))),
        "wrt": np.ascontiguousarray(f("w_router")[0]), "brt": _bc(f("b_router")[0]),
        "bgu": np.ascontiguousarray(bgu), "bdn": np.ascontiguousarray(f("b_down")[0]),
        "wgu": np.ascontiguousarray(f("w_gu")[0]), "wdn": np.ascontiguousarray(f("w_down")[0]),
    }
    perm = np.concatenate([np.concatenate([np.arange(r * 128, r * 128 + 128),
                                           np.arange(512 + r * 128, 512 + r * 128 + 128)]) for r in range(4)])
    maps = []
    for c in range(NCORES):
        b, hp = c // 4, c % 4
        cs = slice(hp * 128, hp * 128 + 128)
        hsl = np.arange(hp * 128, hp * 128 + 128)
        fcols = np.concatenate([hsl, 512 + hsl, 1024 + hsl])
        rcols = np.concatenate([R0 + hsl, R0 + 512 + hsl, R0 + 1024 + hsl, R0 + 1536 + np.arange(256)])
        mucols = rcols - R0
        rowA = np.zeros((128, NA), np.float32)

        def put(name, v):
            a, b_ = RA[name]
            rowA[:, a:b_] = _bc(v)
        put("wf0", w_in[:, 1536 + 2 * hp])
        put("wf1", w_in[:, 1536 + 2 * hp + 1])
        put("mu", f("rwkv_mu")[0][mucols])
        put("w0", f("w0")[0][cs]); put("a0", f("a0")[0][cs]); put("kk", f("k_k")[0][cs])
        put("ka", f("k_a")[0][cs]); put("rk", f("r_k")[0].reshape(-1)[cs])
        put("lg", f("lnx_g")[0][cs]); put("lb", f("lnx_b")[0][cs]); put("fg", f("fox_norm_g")[0][cs])
        put("bf", f("b_fgate")[0][2 * hp:2 * hp + 2])
        m = dict(shared)
        m["xb"] = np.ascontiguousarray(np.concatenate([tile0, x[b, :NTX * 128]], 0))
        m["rowA"] = rowA
        m["wfox"] = np.ascontiguousarray(w_in[:, fcols])
        m["wrw"] = np.ascontiguousarray(w_in[:, rcols])
        m["w2a2"] = np.ascontiguousarray(np.concatenate([f("w2")[0][:, cs], f("a2")[0][:, cs]], 0))
        m["g2"] = np.ascontiguousarray(f("g2")[0][:, cs])
        m["woutc"] = np.ascontiguousarray(np.concatenate([w_out[hp * 128:hp * 128 + 128],
                                                          w_out[512 + hp * 128:512 + hp * 128 + 128]], 0))
        TPC = NTX // 4
        m["xc"] = np.ascontiguousarray(x[b, hp * TPC * 128:(hp + 1) * TPC * 128])
        maps.append(m)
    return maps


_NC_CACHE = {}


def kernel(**inputs):
    NTX = 64
    if NTX not in _NC_CACHE:
        _NC_CACHE[NTX] = build(NTX, stage="full")
    nc = _NC_CACHE[NTX]
    maps = prep(inputs, NTX)
    res = run_bass_kernel_spmd(nc, maps, core_ids=list(range(NCORES)))
    TPC = NTX // 4
    out = np.zeros((2, NTX * 128, D), np.float32)
    for c in range(NCORES):
        b, hp = c // 4, c % 4
        out[b, hp * TPC * 128:(hp + 1) * TPC * 128] = np.asarray(res.results[c]["out"], np.float32)
    return out
```

```python
from contextlib import ExitStack
import numpy as np
import ml_dtypes
import concourse.bass as bass
import concourse.mybir as mybir
from concourse.bass_utils import run_bass_kernel_spmd

F32 = mybir.dt.float32
BF16 = mybir.dt.bfloat16
AF = mybir.ActivationFunctionType
ALU = mybir.AluOpType
AX = mybir.AxisListType

D = 1024
NCORES = 8
LN_EPS = 1e-5
GN_EPS = 64e-5
RMS_EPS = 1e-6
ALPHA = float(2 ** 0.25)
NEXP = 32
SWL = 7.0
SWA = 1.702
NEGPAD = -30000.0

SAME_ENGINE_SYNC = True


class View:
    __slots__ = ("t", "ap")

    def __init__(self, t, ap):
        self.t = t
        self.ap = ap


class Tl:
    __slots__ = ("h", "w", "rs", "name", "excl")

    def __init__(self, h, name="", excl=False):
        self.excl = excl
        self.h = h
        self.w = None
        self.rs = {}
        self.name = name

    def __getitem__(self, idx):
        return View(self, self.h[idx])

    def v(self, ap):
        return View(self, ap)


class Sched:
    def __init__(self, nc, n_dma_sems=48):
        self.nc = nc
        self.eng = {"pe": nc.tensor, "act": nc.scalar, "dve": nc.vector,
                    "pool": nc.gpsimd, "sp": nc.sync}
        self.sem = {k: nc.alloc_semaphore(name="s_" + k) for k in self.eng}
        self.cnt = {k: 0 for k in self.eng}
        self.seen = {k: {} for k in self.eng}
        self.dsems = [nc.alloc_semaphore(name="d%d" % i) for i in range(n_dma_sems)]
        self.dval = [0] * n_dma_sems
        self.dnext = 0
        self.dnext_p = 0
        self.n_wait = 0
        self.n_inst = 0
        self.clk = {k: 0.0 for k in self.eng}
        self.tfin = {}
        self.last_start = 0.0

    def _wait(self, e, dep):
        if dep is None:
            return
        kind, key, val = dep
        if kind == "e":
            if key == e and (e in ("pe", "sp") or not SAME_ENGINE_SYNC):
                return
            sem = self.sem[key]
        else:
            sem = self.dsems[key]
        k = (kind, key)
        if self.seen[e].get(k, 0) >= val:
            return
        self.eng[e].wait_ge(sem, val)
        self.seen[e][k] = val
        self.n_wait += 1

    def _deps(self, e, reads, writes):
        for t in reads:
            self._wait(e, t.w)
            if t.excl:
                for (kd, ky), v in t.rs.items():
                    if ky != e:
                        self._wait(e, (kd, ky, v))
        for t in writes:
            self._wait(e, t.w)
            for (kd, ky), v in t.rs.items():
                self._wait(e, (kd, ky, v))

    def _commit(self, dep, reads, writes):
        k = (dep[0], dep[1])
        for t in reads:
            if t.rs.get(k, 0) < dep[2]:
                t.rs[k] = dep[2]
        for t in writes:
            t.w = dep
            t.rs = {}

    def _est(self, e, reads, writes, cost):
        st = self.clk[e]
        for t in list(reads) + list(writes):
            f = self.tfin.get(id(t))
            if f is not None and f + 0.15 > st:
                st = f + 0.15
        fin = st + cost
        self.clk[e] = fin if e != "sp" else st + 0.1
        for t in list(reads) + list(writes):
            if self.tfin.get(id(t), 0.0) < fin:
                self.tfin[id(t)] = fin
        self.last_start = st

    def do(self, e, fn, reads=(), writes=(), n=128, f32=True):
        self._deps(e, reads, writes)
        ins = fn(self.eng[e])
        self.cnt[e] += 1
        ins.then_inc(self.sem[e], 1)
        self._commit(("e", e, self.cnt[e]), reads, writes)
        self.n_inst += 1
        if e == "pe":
            cost = (0.10 + n * 0.00085) if f32 else (0.05 + n * 0.00075)
        elif e == "act":
            cost = 0.2 + n * 0.001
        elif e == "dve":
            cost = 0.15 + n * 0.0009
        else:
            cost = 0.2 + n * 0.0021
        self._est(e, reads, writes, cost)
        return ins

    def dma(self, q, out, in_, **kw):
        reads, writes = [in_.t], [out.t]
        self._deps(q, reads, writes)
        nh = len(self.dsems) // 2
        if q == "pool":
            i = nh + self.dnext_p
            self.dnext_p = (self.dnext_p + 1) % (len(self.dsems) - nh)
        else:
            i = self.dnext
            self.dnext = (self.dnext + 1) % nh
        if self.dval[i] > 0:
            self._wait(q, ("d", i, self.dval[i]))
        ins = self.eng[q].dma_start(out=out.ap, in_=in_.ap, **kw)
        self.dval[i] += 16
        ins.then_inc(self.dsems[i], 16)
        self._commit(("d", i, self.dval[i]), reads, writes)
        self.n_inst += 1
        self._est("sp", reads, writes, 2.5)

    def barrier(self):
        for e in self.eng:
            for k in self.eng:
                if k != e and self.cnt[k] > 0:
                    self._wait(e, ("e", k, self.cnt[k]))
            for i, v in enumerate(self.dval):
                if v > 0:
                    self._wait(e, ("d", i, v))

    def wait_all(self, e, tiles):
        for t in tiles:
            self._wait(e, t.w)

    @staticmethod
    def _fs(v):
        n = 1
        for d in v.ap.shape[1:]:
            n *= d
        return n

    def cp(self, e, o, a):
        if e == "act":
            return self.do("act", lambda g: g.copy(out=o.ap, in_=a.ap), [a.t], [o.t], n=self._fs(o))
        return self.do(e, lambda g: g.tensor_copy(out=o.ap, in_=a.ap), [a.t], [o.t], n=self._fs(o))

    def tt(self, e, o, a, b, op):
        return self.do(e, lambda g: g.tensor_tensor(out=o.ap, in0=a.ap, in1=b.ap, op=op),
                       [a.t, b.t], [o.t], n=self._fs(o))

    def ts(self, e, o, a, s1, op0, s2=None, op1=None):
        rd = [a.t]
        s1a, s2a = s1, s2
        if isinstance(s1, View):
            rd.append(s1.t)
            s1a = s1.ap
        if isinstance(s2, View):
            rd.append(s2.t)
            s2a = s2.ap
        if op1 is None:
            return self.do(e, lambda g: g.tensor_scalar(out=o.ap, in0=a.ap, scalar1=s1a, scalar2=None,
                                                        op0=op0), rd, [o.t], n=self._fs(o))
        return self.do(e, lambda g: g.tensor_scalar(out=o.ap, in0=a.ap, scalar1=s1a, scalar2=s2a,
                                                    op0=op0, op1=op1), rd, [o.t], n=self._fs(o))

    def stt(self, e, o, a, s, b, op0, op1):
        rd = [a.t, b.t]
        sa = s
        if isinstance(s, View):
            rd.append(s.t)
            sa = s.ap
        return self.do(e, lambda g: g.scalar_tensor_tensor(out=o.ap, in0=a.ap, scalar=sa, in1=b.ap,
                                                           op0=op0, op1=op1), rd, [o.t], n=self._fs(o))

    def act(self, o, a, func, bias=None, scale=None, accum=None):
        rd = [a.t]
        wr = [o.t]
        kw = {}
        if bias is not None:
            if isinstance(bias, View):
                rd.append(bias.t)
                kw["bias"] = bias.ap
            else:
                kw["bias"] = bias
        if scale is not None:
            if isinstance(scale, View):
                rd.append(scale.t)
                kw["scale"] = scale.ap
            else:
                kw["scale"] = scale
        if accum is not None:
            wr.append(accum.t)
            kw["accum_out"] = accum.ap
        return self.do("act", lambda g: g.activation(out=o.ap, in_=a.ap, func=func, **kw), rd, wr,
                       n=self._fs(o))

    def mm(self, o, l, r, start, stop):
        return self.do("pe", lambda g: g.matmul(o.ap, lhsT=l.ap, rhs=r.ap, start=start, stop=stop),
                       [l.t, r.t], [o.t], n=max(64, self._fs(r)), f32=(r.ap.dtype == F32))

    def tr(self, o, a, ident):
        return self.do("pe", lambda g: g.transpose(out=o.ap, in_=a.ap, identity=ident.ap),
                       [a.t, ident.t], [o.t], n=128, f32=(a.ap.dtype == F32))

    def red(self, e, o, a, op=ALU.add):
        return self.do(e, lambda g: g.tensor_reduce(out=o.ap, in_=a.ap, axis=AX.X, op=op), [a.t], [o.t],
                       n=self._fs(a))


RA = {}
_o = 0
for _n, _s in [("wf0", 1024), ("wf1", 1024), ("mu", 640), ("w0", 128), ("a0", 128), ("kk", 128),
               ("ka", 128), ("rk", 128), ("lg", 128), ("lb", 128), ("fg", 128), ("bf", 2)]:
    RA[_n] = (_o, _o + _s)
    _o += _s
NA = _o
CM = {"ident": 0, "miu": 1, "msu": 2, "msl": 3, "ones": 4}


def build(NTX, stage="full", cut=99):
    NT = NTX + 1
    TPC = NTX // 4
    NTOK = TPC * 128
    nc = bass.Bass("TRN2", target_bir_lowering=False)

    def din(name, shape, dt=F32):
        return Tl(nc.dram_tensor(name, shape, dt, kind="ExternalInput").ap(), name)

    def dout(name, shape, dt=F32):
        return Tl(nc.dram_tensor(name, shape, dt, kind="ExternalOutput").ap(), name)

    xb = din("xb", [NT * 128, D])
    lngb = din("lngb", [128, 6, D])
    rowA_d = din("rowA", [128, NA])
    wfox_d = din("wfox", [D, 384])
    wrw_d = din("wrw", [D, 640])
    w2a2_d = din("w2a2", [128, 128])
    g2_d = din("g2", [128, 128])
    cmat_d = din("cmat", [128, 5, 128])
    ccol_d = din("ccol", [128, 2])
    woutc_d = din("woutc", [256, D])
    rsin = Tl(nc.dram_tensor("rsin", [NTX * 128, D], F32).ap(), "rsin")
    rsout = Tl(nc.dram_tensor("rsout", [NTOK, D], F32).ap(), "rsout")
    if stage != "A":
        xc_d = din("xc", [NTOK, D])
        iota_d = din("iota", [128, 512])
        wrt_d = din("wrt", [D, NEXP])
        brt_d = din("brt", [128, NEXP])
        bgu_d = din("bgu", [128, NEXP * 16])
        bdn_d = din("bdn", [NEXP, D])
        wgu_d = din("wgu", [NEXP, D, 2 * D])
        wdn_d = din("wdn", [NEXP, D, D])
        out_d = dout("out", [NTOK, D])
    gin = Tl(nc.dram_tensor("gin", [NTX * 128, 256], BF16).ap(), "gin")
    gout = Tl(nc.dram_tensor("gout", [NCORES * NTX * 128, 256], BF16).ap(), "gout")
    h1_d = Tl(nc.dram_tensor("h1s", [NTOK, D], F32).ap(), "h1s")
    dbg = None
    if stage != "full":
        dbg = dout("dbg", [NTX * 128, 256], BF16)

    S = Sched(nc)

    with ExitStack() as es0:
        def sb(es, name, shape, dt=F32):
            return Tl(es.enter_context(nc.sbuf_tensor("sb_" + name, shape, dt)), name)

        cm = sb(es0, "cm", [128, 5, 128])
        identb = sb(es0, "identb", [128, 128], BF16)
        ln0 = sb(es0, "ln0", [128, 2, D])
        S.dma("sp", cm[:], cmat_d[:])
        S.dma("sp", ln0[:], lngb[:, 0:2, :])
        S.cp("dve", identb[:], cm[:, 0, :])
        ident = cm[:, 0, :]
        MIU, MSU, MSL, ONES = cm[:, 1, :], cm[:, 2, :], cm[:, 3, :], cm[:, 4, :]
        P = [Tl(es0.enter_context(nc.psum_tensor("pb%d" % i, [128, 512], F32)), "pb%d" % i, excl=True)
             for i in range(7)]
        PTb = Tl(es0.enter_context(nc.psum_tensor("ptb", [128, 1024], BF16)), "ptb", excl=True)

        def layer_norm_stats(es_tmp, src, tagname, st6, mv, rs, nmr, eps, width=D):
            nchunk = (width + 511) // 512
            for c in range(nchunk):
                w0_, w1_ = c * 512, min(width, (c + 1) * 512)
                S.do("dve", lambda g: g.bn_stats(out=st6.h[:, c, :], in_=src.ap[:, w0_:w1_]),
                     [src.t], [st6])
            S.do("dve", lambda g: g.bn_aggr(out=mv.h[:, :], in_=st6.h[:, 0:nchunk, :]), [st6], [mv])
            S.act(rs[:, 0:1], mv[:, 1:2], AF.Ln, bias=eps)
            S.act(rs[:, 0:1], rs[:, 0:1], AF.Exp, scale=-0.5)
            S.ts("dve", nmr[:, 0:1], mv[:, 0:1], rs[:, 0:1], ALU.mult, -1.0, ALU.mult)

        with ExitStack() as esA:
            rowA = sb(esA, "rowA", [128, NA])
            ccol = sb(esA, "ccol", [128, 2])
            wfox = sb(esA, "wfox", [128, 8, 384], BF16)
            w1 = sb(esA, "w1", [128, 8, 640], BF16)
            w2 = sb(esA, "w2", [128, 8, 640], BF16)
            w2a2 = sb(esA, "w2a2", [128, 128], BF16)
            g2 = sb(esA, "g2", [128, 128], BF16)
            S.dma("sp", rowA[:], rowA_d[:])
            S.dma("sp", ccol[:], ccol_d[:])
            S.dma("pool", wfox[:], wfox_d.v(wfox_d.h.rearrange("(k p) n -> p k n", p=128)))
            S.dma("pool", w2a2[:], w2a2_d[:])
            S.dma("pool", g2[:], g2_d[:])

            def ra(name):
                a, b = RA[name]
                return rowA[:, a:b]

            with ExitStack() as esW:
                wrwf = sb(esW, "wrwf", [128, 8, 640])
                tmpw = sb(esW, "tmpw", [128, 640])
                S.dma("sp", wrwf[:], wrw_d.v(wrw_d.h.rearrange("(k p) n -> p k n", p=128)))
                for k in range(8):
                    S.tt("dve", tmpw[:], wrwf[:, k, :], ra("mu"), ALU.mult)
                    S.cp("act", w2[:, k, :], tmpw[:])
                    S.tt("dve", w1[:, k, :], wrwf[:, k, :], tmpw[:], ALU.subtract)
                S.barrier()

            qT_h = esA.enter_context(nc.sbuf_tensor("qT", [128, NT * 128], BF16))
            kT_h = esA.enter_context(nc.sbuf_tensor("kT", [128, NT * 128], BF16))
            V_h = esA.enter_context(nc.sbuf_tensor("Vall", [128, NT, 2, 65], BF16))
            qT = [Tl(qT_h[:, i * 128:(i + 1) * 128], "qT%d" % i) for i in range(NT)]
            kT = [Tl(kT_h[:, i * 128:(i + 1) * 128], "kT%d" % i) for i in range(NT)]
            Vt = [Tl(V_h[:, i, :, :], "V%d" % i) for i in range(NT)]
            Vall = Tl(V_h, "Vall")
            S.do("pool", lambda g: g.memset(V_h[:], 1.0), [], [Vall] + Vt)
            negc = sb(esA, "negc", [128, 2, NT])
            cend = sb(esA, "cend", [128, 2])
            cendAll = sb(esA, "cendAll", [128, 2, NT])
            S.do("pool", lambda g: g.memset(cend.h[:], 0.0), [], [cend])
            Hs = sb(esA, "Hs", [128, 64])
            S.do("pool", lambda g: g.memset(Hs.h[:], 0.0), [], [Hs])
            h0T = [sb(esA, "h0T%d" % j, [128, 8, 128], BF16) for j in range(2)]
            h0P = sb(esA, "h0P", [128, 8, 128], BF16)
            lastc = sb(esA, "lastc", [128, 8, 1], BF16)
            S.do("pool", lambda g: g.memset(lastc.h[:], 0.0), [], [lastc])
            xbuf = [sb(esA, "xt%d" % j, [128, D]) for j in range(2)]
            h0b = sb(esA, "h0b", [128, D], BF16)
            junk = sb(esA, "junk", [128, D])
            st6 = sb(esA, "st6", [128, 2, 6])
            mv = sb(esA, "mv", [128, 2])
            rs = sb(esA, "rs", [128, 1])
            nmr = sb(esA, "nmr", [128, 1])
            fl = sb(esA, "fl", [128, 2])
            ctile = sb(esA, "ctile", [128, 2])
            loT = sb(esA, "loT", [128, 256], BF16)
            rkv = sb(esA, "rkv", [128, 384])
            W = {}
            for nm in ["xw", "lw", "a", "g", "kkr", "sq", "kk", "t1", "kmod", "b", "rk", "bonus",
                       "cumS", "d2", "d4", "E1", "E2", "E3", "E4", "Ke", "Be", "RHS", "nU", "y", "yn"]:
                W[nm] = sb(esA, "w_" + nm, [128, 128])
            TM = sb(esA, "TM", [128, 4, 128])
            TT = sb(esA, "TT", [128, 4, 128])
            AT = [sb(esA, "AT%d" % h, [128, 4, 128]) for h in range(2)]
            Zb = [sb(esA, "Z%d" % j, [128, 2, 128]) for j in range(2)]
            Ztb = [sb(esA, "Zt%d" % j, [128, 2, 128]) for j in range(2)]
            TTi = [sb(esA, "TTi%d" % j, [128, 2, 128]) for j in range(2)]
            sm = sb(esA, "sm", [128, 16])
            gC = sb(esA, "gC", [128, 1])
            biasT = sb(esA, "biasT", [128, NT])
            PT = [sb(esA, "PT%d" % j, [128, 128], BF16) for j in range(3)]
            mask_b = sb(esA, "mask_b", [128, 128], BF16)
            S.cp("dve", mask_b[:], MIU)
            mixt = sb(esA, "mixt", [128, 256], BF16)
            mixT = sb(esA, "mixT", [128, 2, 128], BF16)
            pout = sb(esA, "pout", [128, D])
            woutc = sb(esA, "woutc", [128, 2, D], BF16)
            S.dma("pool", woutc[:], woutc_d.v(woutc_d.h.rearrange("(k p) n -> p k n", p=128)))
            osb = sb(esA, "osb", [128, 65])
            maskA = sb(esA, "maskA", [128, 4, 128])
            S.cp("dve", maskA[:, 0, :], MSU)
            S.cp("dve", maskA[:, 1, :], MIU)
            S.cp("dve", maskA[:, 2, :], MSU)
            S.cp("dve", maskA[:, 3, :], MIU)
            ptc = 0


            rkvS = [rkv, sb(esA, "rkv1", [128, 384]), sb(esA, "rkv2", [128, 384])]
            WS = [{}, {}, {}]
            for nm in ["lw", "a", "g", "kk", "kmod", "b", "bonus"]:
                WS[0][nm] = W[nm]
                WS[1][nm] = sb(esA, "w1_" + nm, [128, 128])
                WS[2][nm] = sb(esA, "w2_" + nm, [128, 128])
            LS = []
            for q_ in range(2):
                L_ = {}
                if q_ == 0:
                    L_.update(TM=TM, TT=TT, AT=AT, Zb=Zb, Ztb=Ztb, TTi=TTi, gC=gC)
                    for nm in ["cumS", "d2", "d4", "E1", "E2", "E3", "E4", "Ke", "Be"]:
                        L_[nm] = W[nm]
                else:
                    L_["TM"] = sb(esA, "TM_1", [128, 4, 128])
                    L_["TT"] = sb(esA, "TT_1", [128, 4, 128])
                    L_["AT"] = [sb(esA, "AT%d_1" % h, [128, 4, 128]) for h in range(2)]
                    L_["Zb"] = [sb(esA, "Z%d_1" % j, [128, 2, 128]) for j in range(2)]
                    L_["Ztb"] = [sb(esA, "Zt%d_1" % j, [128, 2, 128]) for j in range(2)]
                    L_["TTi"] = [sb(esA, "TTi%d_1" % j, [128, 2, 128]) for j in range(2)]
                    L_["gC"] = sb(esA, "gC_1", [128, 1])
                    for nm in ["cumS", "d2", "d4", "E1", "E2", "E3", "E4", "Ke", "Be"]:
                        L_[nm] = sb(esA, "L1_" + nm, [128, 128])
                LS.append(L_)
            smP = sb(esA, "smP", [128, 8])
            smR = sb(esA, "smR", [128, 8])
            smA = [sb(esA, "smA%d" % h, [128, 2]) for h in range(2)]
            st6g = sb(esA, "st6g", [128, 2, 6])
            junkA = sb(esA, "junkA", [128, 64])
            biasTh = [biasT, sb(esA, "biasT1", [128, NT])]
            osbh = [osb, sb(esA, "osb1", [128, 65])]
            ptc_box = [0]
            qbd = [sb(esA, "qbd%d" % j, [128, 256], BF16) for j in range(3)]
            for j in range(3):
                S.do("pool", lambda g: g.memset(qbd[j].h[:], 0.0), [], [qbd[j]])
            zrow = sb(esA, "zrow", [128, 256], BF16)
            S.do("pool", lambda g: g.memset(zrow.h[:], 0.0), [], [zrow])
            PT6 = PT + [sb(esA, "PT%d" % j, [128, 128], BF16) for j in range(3, 6)]

            def g_proj(i):
                cur = h0T[i % 2]
                xt = xbuf[i % 2]
                Wp = WS[i % 3]
                rkv_ = rkvS[i % 3]
                if i + 1 < NT:
                    S.dma("sp", xbuf[(i + 1) % 2][:], xb[(i + 1) * 128:(i + 2) * 128, :])
                layer_norm_stats(None, xt[:], "ln0", st6, mv, rs, nmr, LN_EPS)
                yield
                S.act(xt[:], xt[:], AF.Identity, bias=nmr[:, 0:1], scale=rs[:, 0:1])
                S.tt("dve", xt[:], xt[:], ln0[:, 0, :], ALU.mult)
                S.tt("dve", xt[:], xt[:], ln0[:, 1, :], ALU.add)
                if i == 0:
                    S.ts("dve", xt[:], xt[:], ccol[:, 0:1], ALU.mult)
                S.cp("act", h0b[:], xt[:])
                yield
                for h in range(2):
                    a_, b_ = RA["wf%d" % h]
                    S.do("dve", lambda g: g.scalar_tensor_tensor(
                        out=junk.h[:], in0=xt.h[:], scalar=1.0, in1=rowA.h[:, a_:b_],
                        op0=ALU.mult, op1=ALU.mult, accum_out=fl.h[:, h:h + 1]), [xt, rowA], [junk, fl])
                S.tt("dve", fl[:], fl[:], ra("bf"), ALU.add)
                S.act(fl[:], fl[:], AF.Exp, scale=-1.0)
                S.act(fl[:], fl[:], AF.Ln, bias=1.0)
                if i == 0:
                    S.ts("dve", fl[:], fl[:], ccol[:, 0:1], ALU.mult)
                yield
                S.mm(P[1][:, 384:386], MIU, fl[:], True, True)
                S.mm(P[1][:, 386:388], ONES, fl[:], True, True)
                S.tt("dve", negc[:, :, i], P[1][:, 384:386], cend[:], ALU.add)
                S.tt("dve", cend[:], cend[:], P[1][:, 386:388], ALU.add)
                S.cp("dve", cendAll[:, :, i], cend[:])
                if i == 0:
                    S.ts("dve", negc[:, :, 0], negc[:, :, 0], ccol[:, 1:2], ALU.add)
                yield
                for k in range(8):
                    S.tr(PTb[:, k * 128:(k + 1) * 128], h0b[:, k * 128:(k + 1) * 128], identb[:])
                S.cp("act", cur[:], PTb.v(PTb.h[:].rearrange("p (k t) -> p k t", k=8)))
                S.cp("dve", h0P[:, :, 1:128], cur[:, :, 0:127])
                S.cp("pool", h0P[:, :, 0:1], lastc[:])
                S.cp("pool", lastc[:], cur[:, :, 127:128])
                yield
                for k in range(8):
                    S.mm(P[1][:, 0:128], wfox[:, k, 0:128], cur[:, k, :], k == 0, k == 7)
                for k in range(8):
                    S.mm(P[1][:, 128:256], wfox[:, k, 128:256], cur[:, k, :], k == 0, k == 7)
                yield
                for k in range(8):
                    S.mm(P[1][:, 256:384], cur[:, k, :], wfox[:, k, 256:384], k == 0, k == 7)
                S.cp("act", qT[i][:], P[1][:, 0:128])
                S.cp("act", kT[i][:], P[1][:, 128:256])
                S.cp("dve", qbd[i % 3][0:64, 0:128], P[1][0:64, 0:128])
                S.cp("dve", qbd[i % 3][64:128, 128:256], P[1][64:128, 0:128])
                S.cp("act", Vt[i].v(Vt[i].h[:, :, 0:64]),
                     P[1].v(P[1].h[:, 256:384].rearrange("p (h d) -> p h d", h=2)))
                yield
                for k in range(8):
                    S.mm(P[1][:, 0:384], cur[:, k, :], w1[:, k, 0:384], k == 0, False)
                    S.mm(P[1][:, 0:384], h0P[:, k, :], w2[:, k, 0:384], False, k == 7)
                S.cp("act", rkv_[:], P[1][:, 0:384])
                yield
                for grp in range(2):
                    c0 = 384 + grp * 128
                    for k in range(8):
                        S.mm(P[1][:, grp * 128:(grp + 1) * 128], w1[:, k, c0:c0 + 128], cur[:, k, :],
                             k == 0, False)
                        S.mm(P[1][:, grp * 128:(grp + 1) * 128], w2[:, k, c0:c0 + 128], h0P[:, k, :],
                             False, k == 7)
                    yield
                S.act(W["sq"][0:64, :], P[1][0:64, 0:128], AF.Exp, scale=-2.0)
                S.cp("act", loT[64:128, 0:128], P[1][64:128, 0:128])
                S.act(W["xw"][:], P[1][:, 128:256], AF.Exp, scale=-1.0)
                S.ts("dve", W["sq"][0:64, :], W["sq"][0:64, :], 1.0, ALU.add)
                S.do("dve", lambda g: g.reciprocal(out=W["sq"].h[0:64, :], in_=W["sq"].h[0:64, :]), [W["sq"]], [W["sq"]])
                S.ts("dve", loT[0:64, 0:128], W["sq"][0:64, :], 2.0, ALU.mult, -1.0, ALU.add)
                S.ts("dve", W["xw"][:], W["xw"][:], 1.0, ALU.add)
                S.do("dve", lambda g: g.reciprocal(out=W["xw"].h[:], in_=W["xw"].h[:]), [W["xw"]], [W["xw"]])
                S.cp("act", loT[:, 128:256], W["xw"][:])
                S.mm(P[1][:, 0:128], loT[0:64, 0:128], w2a2[0:64, :], True, True)
                S.mm(P[1][:, 128:256], loT[:, 128:256], g2[:, :], True, True)
                S.mm(P[1][:, 256:384], loT[64:128, 0:128], w2a2[64:128, :], True, True)
                yield
                r_, kraw, v_ = rkv_[:, 0:128], rkv_[:, 128:256], rkv_[:, 256:384]
                S.tt("dve", W["xw"][:], P[1][:, 0:128], ra("w0"), ALU.add)
                S.act(W["xw"][:], W["xw"][:], AF.Exp, scale=-1.0)
                S.ts("dve", W["xw"][:], W["xw"][:], 1.0, ALU.add)
                S.do("dve", lambda g: g.reciprocal(out=Wp["lw"].h[:], in_=W["xw"].h[:]), [W["xw"]], [Wp["lw"]])
                S.ts("dve", Wp["lw"][:], Wp["lw"][:], -0.6065306597126334, ALU.mult)
                S.tt("dve", W["t1"][:], P[1][:, 256:384], ra("a0"), ALU.add)
                S.act(W["t1"][:], W["t1"][:], AF.Exp, scale=-1.0)
                S.ts("dve", W["t1"][:], W["t1"][:], 1.0, ALU.add)
                S.do("dve", lambda g: g.reciprocal(out=Wp["a"].h[:], in_=W["t1"].h[:]), [W["t1"]], [Wp["a"]])
                S.cp("act", Wp["g"][:], P[1][:, 128:256])
                yield
                S.tt("dve", W["kkr"][:], kraw, ra("kk"), ALU.mult)
                S.act(W["sq"][:], W["kkr"][:], AF.Square)
                S.red("dve", smP[:, 0:2], W["sq"].v(W["sq"].h[:].rearrange("p (h n) -> p h n", h=2)))
                S.ts("dve", smP[:, 0:2], smP[:, 0:2], 1e-16, ALU.max)
                S.act(smP[:, 2:4], smP[:, 0:2], AF.Ln)
                S.act(smP[:, 2:4], smP[:, 2:4], AF.Exp, scale=-0.5)
                for h in range(2):
                    S.ts("dve", Wp["kk"][:, h * 64:(h + 1) * 64], W["kkr"][:, h * 64:(h + 1) * 64],
                         smP[:, 2 + h:3 + h], ALU.mult)
                yield
                S.stt("dve", W["t1"][:], Wp["a"][:], -1.0, ra("ka"), ALU.add, ALU.mult)
                S.stt("dve", Wp["kmod"][:], W["t1"][:], 1.0, kraw, ALU.add, ALU.mult)
                S.tt("pool", Wp["b"][:], Wp["kk"][:], Wp["a"][:], ALU.mult)
                S.tt("pool", W["rk"][:], r_, Wp["kmod"][:], ALU.mult)
                S.tt("pool", W["rk"][:], W["rk"][:], ra("rk"), ALU.mult)
                S.red("dve", smP[:, 4:6], W["rk"].v(W["rk"].h[:].rearrange("p (h n) -> p h n", h=2)))
                for h in range(2):
                    S.ts("dve", Wp["bonus"][:, h * 64:(h + 1) * 64], rkv_[:, 256 + h * 64:320 + h * 64],
                         smP[:, 4 + h:5 + h], ALU.mult)
                yield

            def g_local(i):
                Wp = WS[i % 3]
                rkv_ = rkvS[i % 3]
                L = LS[i % 2]
                TM_, TT_, AT_, Zb_, Ztb_, TTi_ = L["TM"], L["TT"], L["AT"], L["Zb"], L["Ztb"], L["TTi"]
                r_ = rkv_[:, 0:128]
                lw = Wp["lw"]
                S.mm(P[5][:, 0:128], MIU, lw[:], True, True)
                S.mm(P[5][:, 128:256], ONES, lw[:], True, True)
                S.mm(P[5][:, 256:257], lw[:], ONES.t.v(cm.h[:, 4, 0:1]), True, True)
                S.cp("dve", L["cumS"][:], P[5][:, 0:128])
                S.tt("dve", L["d4"][:], P[5][:, 128:256], L["cumS"][:], ALU.subtract)
                S.act(L["gC"][:], P[5][:, 256:257], AF.Exp)
                yield
                S.act(L["E1"][:], L["cumS"][:], AF.Exp)
                S.act(L["E3"][:], L["cumS"][:], AF.Exp, scale=-1.0)
                S.tt("dve", L["d2"][:], L["cumS"][:], lw[:], ALU.subtract)
                S.act(L["E2"][:], L["d2"][:], AF.Exp)
                S.act(L["E4"][:], L["d4"][:], AF.Exp)
                yield
                S.tt("pool", TM_[:, 0, :], Wp["kk"][:], L["E2"][:], ALU.mult)
                S.tt("dve", TM_[:, 1, :], r_, L["E1"][:], ALU.mult)
                S.tt("dve", TM_[:, 2, :], Wp["kmod"][:], L["E3"][:], ALU.mult)
                S.tt("dve", TM_[:, 3, :], Wp["b"][:], L["E3"][:], ALU.mult)
                S.tt("pool", L["Ke"][:], Wp["kmod"][:], L["E4"][:], ALU.mult)
                S.tt("pool", L["Be"][:], Wp["b"][:], L["E4"][:], ALU.mult)
                yield
                for j in range(4):
                    S.tr(P[6][:, j * 128:(j + 1) * 128], TM_[:, j, :], ident)
                S.cp("act", TT_[:], P[6].v(P[6].h[:].rearrange("p (j t) -> p j t", j=4)))
                yield
                for h in range(2):
                    hs = slice(h * 64, (h + 1) * 64)
                    S.mm(P[5][:, 0:256], TT_[hs, 2, :], TT_.v(TT_.h[hs, 0:2, :]), True, True)
                    S.mm(P[5][:, 256:512], TT_[hs, 3, :], TT_.v(TT_.h[hs, 0:2, :]), True, True)
                    S.tt("dve", AT_[h][:], P[5].v(P[5].h[:].rearrange("p (j t) -> p j t", j=4)), maskA[:],
                         ALU.mult)
                    S.mm(P[6][:, h * 128:(h + 1) * 128], TT_[hs, 0, :], TT_[hs, 3, :], True, True)
                    yield
                S.tt("dve", Ztb_[0][:], P[6].v(P[6].h[:, 0:256].rearrange("p (h t) -> p h t", h=2)),
                     MSL.t.v(cm.h[:, 3:4, :].to_broadcast([128, 2, 128])), ALU.mult)
                for h in range(2):
                    S.cp("act", Zb_[0][:, h, :], AT_[h][:, 2, :])
                    S.tt("pool", TTi_[0][:, h, :], ident, AT_[h][:, 2, :], ALU.subtract)
                yield
                zc = 0
                for lev in range(6):
                    zn = 1 - zc
                    last = lev == 5
                    for h in range(2):
                        S.mm(P[5][:, 256 + h * 128:384 + h * 128], Zb_[zc][:, h, :], Ztb_[zc][:, h, :], True, True)
                    if not last:
                        for h in range(2):
                            S.mm(P[5][:, h * 128:(h + 1) * 128], Ztb_[zc][:, h, :], Zb_[zc][:, h, :], True, True)
                    S.cp("dve", Ztb_[zn][:], P[5].v(P[5].h[:, 256:512].rearrange("p (h t) -> p h t", h=2)))
                    if not last:
                        S.cp("act", Zb_[zn][:], P[5].v(P[5].h[:, 0:256].rearrange("p (h t) -> p h t", h=2)))
                    yield
                    for h in range(2):
                        S.mm(P[6][:, h * 128:(h + 1) * 128], Ztb_[zn][:, h, :], TTi_[zc][:, h, :], True, True)
                    S.tt("dve", TTi_[zn][:], TTi_[zc][:],
                         P[6].v(P[6].h[:, 0:256].rearrange("p (h t) -> p h t", h=2)), ALU.add)
                    zc = zn
                    yield
                assert zc == 0

            def g_state(i):
                rkv_ = rkvS[i % 3]
                L = LS[i % 2]
                TT_, AT_, Tinv = L["TT"], L["AT"], L["TTi"][0]
                for h in range(2):
                    hs = slice(h * 64, (h + 1) * 64)
                    cs = slice(h * 64, (h + 1) * 64)
                    S.mm(P[2][:, cs], TT_[hs, 0, :], Hs[hs, :], True, False)
                    S.mm(P[2][:, cs], AT_[h][:, 0, :], rkv_[:, 256 + h * 64:320 + h * 64], False, True)
                S.cp("act", W["RHS"][:], P[2][:, 0:128])
                yield
                for h in range(2):
                    cs = slice(h * 64, (h + 1) * 64)
                    S.mm(P[2][:, 128 + h * 64:192 + h * 64], Tinv[:, h, :], W["RHS"][:, cs], True, True)
                S.ts("dve", W["nU"][:], P[2][:, 128:256], -1.0, ALU.mult)
                yield
                for h in range(2):
                    hs = slice(h * 64, (h + 1) * 64)
                    cs = slice(h * 64, (h + 1) * 64)
                    vv = rkv_[:, 256 + h * 64:320 + h * 64]
                    if i > 0:
                        S.mm(P[2][:, 256 + h * 64:320 + h * 64], TT_[hs, 1, :], Hs[hs, :], True, False)
                        S.mm(P[2][:, 256 + h * 64:320 + h * 64], AT_[h][:, 1, :], vv, False, False)
                        S.mm(P[2][:, 256 + h * 64:320 + h * 64], AT_[h][:, 3, :], W["nU"][:, cs], False, True)
                    S.mm(P[2][hs, 384:448], L["Ke"][:, cs], vv, True, False)
                    S.mm(P[2][hs, 384:448], L["Be"][:, cs], W["nU"][:, cs], False, True)
                S.stt("dve", Hs[:], Hs[:], L["gC"][:, 0:1], P[2][:, 384:448], ALU.mult, ALU.add)
                if i > 0:
                    S.cp("act", W["y"][:], P[2][:, 256:384])
                yield

            def g_att(i):
                for h in range(2):
                    S.ts("dve", biasTh[h][:, 0:i + 1], negc[:, h, 0:i + 1], cendAll[:, h, i:i + 1], ALU.subtract)
                S.mm(P[0][:, 0:256], zrow[:, 0:128], zrow[:, 0:256], True, False)
                yield
                qb = qbd[i % 3]
                base = ptc_box[0]
                ptc_box[0] += 2 * (i + 1)
                for j in range(i + 2):
                    if j <= i:
                        sbank = P[3] if (j % 2 == 0) else P[4]
                        S.mm(sbank[:, 0:256], kT[j][:], qb[:], True, True)
                    if j >= 1:
                        jp = j - 1
                        for h in range(2):
                            S.mm(P[0][:, h * 128:h * 128 + 65], PT6[(base + 2 * jp + h) % 6][:], Vt[jp][:, h, :],
                                 False, jp == i and h == 1)
                    if j <= i:
                        for h in range(2):
                            pt = PT6[(base + 2 * j + h) % 6]
                            S.act(pt[:], sbank[:, h * 128:(h + 1) * 128], AF.Exp, bias=biasTh[h][:, j:j + 1],
                                  scale=0.125)
                            if j == i:
                                S.tt("pool", pt[:], pt[:], mask_b[:], ALU.mult)
                    yield
                for h in range(2):
                    ob = osbh[h]
                    sA = smA[h]
                    S.cp("act", ob[:], P[0][:, h * 128:h * 128 + 65])
                    S.do("dve", lambda g: g.reciprocal(out=sA.h[:, 0:1], in_=ob.h[:, 64:65]), [ob], [sA])
                    S.ts("dve", ob[:, 0:64], ob[:, 0:64], sA[:, 0:1], ALU.mult)
                    S.act(junkA[:], ob[:, 0:64], AF.Square, accum=sA[:, 1:2])
                    S.ts("dve", sA[:, 1:2], sA[:, 1:2], 1.0 / 64, ALU.mult, RMS_EPS, ALU.add)
                    S.act(sA[:, 1:2], sA[:, 1:2], AF.Ln)
                    S.act(sA[:, 1:2], sA[:, 1:2], AF.Exp, scale=-0.5)
                    a_, b_ = RA["fg"]
                    S.stt("dve", mixt[:, h * 64:(h + 1) * 64], ob[:, 0:64], sA[:, 1:2],
                          rowA[:, a_ + h * 64:a_ + (h + 1) * 64], ALU.mult, ALU.mult)
                    yield

            def post(i):
                Wp = WS[i % 3]
                for h in range(2):
                    cs = slice(h * 64, (h + 1) * 64)
                    S.do("dve", lambda g: g.bn_stats(out=st6g.h[:, 0, :], in_=W["y"].h[:, cs]), [W["y"]], [st6g])
                    S.do("dve", lambda g: g.bn_aggr(out=smR.h[:, 2 * h:2 + 2 * h], in_=st6g.h[:, 0:1, :]),
                         [st6g], [smR])
                for h in range(2):
                    S.act(smR[:, 6 + h:7 + h], smR[:, 1 + 2 * h:2 + 2 * h], AF.Ln, bias=GN_EPS)
                S.act(smR[:, 6:8], smR[:, 6:8], AF.Exp, scale=-0.5)
                for h in range(2):
                    cs = slice(h * 64, (h + 1) * 64)
                    S.ts("dve", W["yn"][:, cs], W["y"][:, cs], smR[:, 2 * h:1 + 2 * h], ALU.subtract,
                         smR[:, 6 + h:7 + h], ALU.mult)
                S.tt("dve", W["yn"][:], W["yn"][:], ra("lg"), ALU.mult)
                S.tt("pool", W["yn"][:], W["yn"][:], ra("lb"), ALU.add)
                S.tt("pool", W["yn"][:], W["yn"][:], Wp["bonus"][:], ALU.add)
                S.tt("dve", mixt[:, 128:256], W["yn"][:], Wp["g"][:], ALU.mult)
                if stage == "A":
                    S.dma("sp", gin[(i - 1) * 128:i * 128, :], mixt[:])
                for kc in range(2):
                    S.tr(PTb[:, kc * 128:(kc + 1) * 128], mixt[:, kc * 128:(kc + 1) * 128], identb[:])
                S.cp("act", mixT[:], PTb.v(PTb.h[:, 0:256].rearrange("p (k t) -> p k t", k=2)))
                for half in range(2):
                    for kc in range(2):
                        S.mm(P[1][:, 0:512], mixT[:, kc, :], woutc[:, kc, half * 512:(half + 1) * 512],
                             kc == 0, kc == 1)
                    S.cp("act", pout[:, half * 512:(half + 1) * 512], P[1][:, 0:512])
                S.dma("sp", rsin[(i - 1) * 128:i * 128, :], pout[:])

            def interleave(gens):
                act_ = [[g_, 0.0] for g_, _ in gens]
                while act_:
                    it = min(act_, key=lambda z: z[1])
                    try:
                        next(it[0])
                        it[1] = S.last_start
                    except StopIteration:
                        act_.remove(it)

            S.dma("sp", xbuf[0][:], xb[0:128, :])
            for _ in g_proj(0):
                pass
            interleave([(g_proj(1), 1), (g_local(0), 1)])
            for i in range(NT):
                gens = [(g_state(i), 1)]
                if i + 1 < NT:
                    gens.append((g_local(i + 1), 1))
                if i > 0:
                    gens.append((g_att(i), 1))
                if i + 2 < NT:
                    gens.append((g_proj(i + 2), 1))
                interleave(gens)
                if i > 0:
                    post(i)


        if stage == "A":
            stg = sb(es0, "stg", [128, NTX, 256], BF16)
            S.dma("sp", stg[:], gin.v(gin.h.rearrange("(n p) c -> p n c", p=128)))
            S.dma("sp", dbg.v(dbg.h.rearrange("(n p) c -> p n c", p=128)), stg[:])
            S.wait_all("sp", [dbg])
            print("insts", S.n_inst, "waits", S.n_wait)
            return nc
        S.barrier()
        cc = nc.alloc_semaphore(name="cc")
        S._deps("pool", [rsin], [rsout])
        nc.gpsimd.collective_compute("ReduceScatter", ALU.add, replica_groups=[[0, 1, 2, 3], [4, 5, 6, 7]],
                                     ins=[rsin.h.opt()], outs=[rsout.h.opt()]).then_inc(cc)
        for e in S.eng:
            S.eng[e].wait_ge(cc, 1)

        GS = min(512, NTOK)
        NTG = NTOK // GS
        with ExitStack() as esC:
            CAP = min(384, NTOK)
            NSC = CAP // 128
            h1bT = sb(esC, "h1bT", [128, TPC, D], BF16)
            posm = sb(esC, "posm", [128, TPC, NEXP])
            carry = sb(esC, "carry", [128, NEXP])
            S.do("pool", lambda g: g.memset(carry.h[:], 0.0), [], [carry])
            yacc = sb(esC, "yacc", [128, TPC, D])
            Gall = sb(esC, "Gall", [128, TPC, NEXP])
            st6 = sb(esC, "c_st6", [128, 2, 6])
            mv = sb(esC, "c_mv", [128, 2])
            rs = sb(esC, "c_rs", [128, 1])
            nmr = sb(esC, "c_nmr", [128, 1])
            with ExitStack() as esC1:
                lnr = sb(esC1, "lnr", [128, 2, D])
                wrt = sb(esC1, "wrt", [128, 8, NEXP])
                brt = sb(esC1, "brt", [128, NEXP])
                S.dma("sp", lnr[:], lngb[:, 2:4, :])
                S.dma("sp", wrt[:], wrt_d.v(wrt_d.h.rearrange("(k p) n -> p k n", p=128)))
                S.dma("sp", brt[:], brt_d[:])
                def c1_bufs(q):
                    return dict(
                        xt=sb(esC1, "c_xt%d" % q, [128, D]), mm=sb(esC1, "c_mm%d" % q, [128, D]),
                        h1=sb(esC1, "c_h1%d" % q, [128, D]), h1Tf=sb(esC1, "c_h1Tf%d" % q, [128, 8, 128]),
                        lg=sb(esC1, "c_lg%d" % q, [128, NEXP]), ex=sb(esC1, "c_ex%d" % q, [128, NEXP]),
                        msk=sb(esC1, "c_msk%d" % q, [128, NEXP]), t8=sb(esC1, "c_t8%d" % q, [128, 8]),
                        sm=sb(esC1, "c_sm%d" % q, [128, 4]), st6=sb(esC1, "c_st6%d" % q, [128, 2, 6]),
                        mv=sb(esC1, "c_mv%d" % q, [128, 2]), rs=sb(esC1, "c_rs%d" % q, [128, 1]),
                        nmr=sb(esC1, "c_nmr%d" % q, [128, 1]))
                CB = [c1_bufs(0), c1_bufs(1)]

                def g_c1(tt):
                    B = CB[tt % 2]
                    pb = 4 * (tt % 2)
                    xt, mm_, h1, h1Tf, lg, ex, msk, t8, sm = (B["xt"], B["mm"], B["h1"], B["h1Tf"], B["lg"],
                                                              B["ex"], B["msk"], B["t8"], B["sm"])
                    rows = slice(tt * 128, (tt + 1) * 128)
                    S.dma("sp", xt[:], xc_d[rows, :])
                    S.dma("sp", mm_[:], rsout[rows, :])
                    layer_norm_stats(None, xt[:], "ln0c", B["st6"], B["mv"], B["rs"], B["nmr"], LN_EPS)
                    yield
                    S.act(xt[:], xt[:], AF.Identity, bias=B["nmr"][:, 0:1], scale=B["rs"][:, 0:1])
                    S.tt("dve", xt[:], xt[:], ln0[:, 0, :], ALU.mult)
                    S.tt("dve", xt[:], xt[:], ln0[:, 1, :], ALU.add)
                    S.stt("dve", xt[:], xt[:], ALPHA, mm_[:], ALU.mult, ALU.add)
                    yield
                    layer_norm_stats(None, xt[:], "ln1", B["st6"], B["mv"], B["rs"], B["nmr"], LN_EPS)
                    yield
                    S.act(xt[:], xt[:], AF.Identity, bias=B["nmr"][:, 0:1], scale=B["rs"][:, 0:1])
                    S.tt("dve", xt[:], xt[:], lnr[:, 0, :], ALU.mult)
                    S.tt("dve", h1[:], xt[:], lnr[:, 1, :], ALU.add)
                    S.dma("sp", h1_d[rows, :], h1[:])
                    S.cp("act", h1bT[:, tt, :], h1[:])
                    yield
                    for grp in range(2):
                        for k4 in range(4):
                            k = grp * 4 + k4
                            S.tr(P[pb + grp][:, k4 * 128:(k4 + 1) * 128], h1[:, k * 128:(k + 1) * 128], ident)
                        S.cp("act", h1Tf[:, grp * 4:(grp + 1) * 4, :],
                             P[pb + grp].v(P[pb + grp].h[:].rearrange("p (k t) -> p k t", k=4)))
                        yield
                    for k in range(8):
                        S.mm(P[pb + 2][:, 0:NEXP], h1Tf[:, k, :], wrt[:, k, :], k == 0, k == 7)
                    S.tt("dve", lg[:], P[pb + 2][:, 0:NEXP], brt[:], ALU.add)
                    S.do("dve", lambda g: g.max(out=t8.h[:], in_=lg.h[:]), [lg], [t8])
                    S.ts("dve", msk[:], lg[:], t8[:, 3:4], ALU.is_ge)
                    S.ts("dve", sm[:, 0:1], t8[:, 0:1], -1.0, ALU.mult)
                    yield
                    S.act(ex[:], lg[:], AF.Exp, bias=sm[:, 0:1])
                    S.tt("dve", ex[:], ex[:], msk[:], ALU.mult)
                    S.red("dve", sm[:, 1:2], ex[:])
                    S.do("dve", lambda g: g.reciprocal(out=sm.h[:, 2:3], in_=sm.h[:, 1:2]), [sm], [sm])
                    S.ts("dve", Gall[:, tt, :], ex[:], sm[:, 2:3], ALU.mult)
                    yield

                def c1_pos(tt):
                    B = CB[tt % 2]
                    pb = 4 * (tt % 2)
                    lg, msk = B["lg"], B["msk"]
                    S.mm(P[pb + 2][:, 64:64 + NEXP], MIU, msk[:], True, True)
                    S.mm(P[pb + 2][:, 128:128 + NEXP], ONES, msk[:], True, True)
                    S.tt("dve", lg[:], P[pb + 2][:, 64:64 + NEXP], carry[:], ALU.add)
                    S.tt("dve", lg[:], lg[:], msk[:], ALU.mult)
                    S.ts("dve", posm[:, tt, :], lg[:], -1.0, ALU.add)
                    S.tt("dve", carry[:], carry[:], P[pb + 2][:, 128:128 + NEXP], ALU.add)

                def interleave_c(gens):
                    act_ = [[g_, 0.0] for g_ in gens]
                    while act_:
                        it = min(act_, key=lambda z: z[1])
                        try:
                            next(it[0])
                            it[1] = S.last_start
                        except StopIteration:
                            act_.remove(it)

                for t0_ in range(0, TPC, 2):
                    tl_ = list(range(t0_, min(TPC, t0_ + 2)))
                    interleave_c([g_c1(t) for t in tl_])
                    for t in tl_:
                        c1_pos(t)
                S.barrier()
            with ExitStack() as esE:
                iota = sb(esE, "iota", [128, 512])
                S.dma("sp", iota[:], iota_d[:])
                bgu = sb(esE, "bgu", [128, NEXP * 16])
                S.dma("sp", bgu[:], bgu_d[:])
                bgu1 = sb(esE, "bgu1", [128, NEXP * 16])
                S.ts("dve", bgu1[:], bgu[:], 1.0, ALU.add)
                Sel = sb(esE, "Sel", [128, TPC, CAP], BF16)
                SelT = sb(esE, "SelT", [128, NSC, NTOK], BF16)
                xeT = sb(esE, "xeT", [128, 8, CAP], BF16)
                actT = sb(esE, "actT", [128, 8, CAP], BF16)
                oe = sb(esE, "oe", [128, NSC, D], BF16)
                NPB = 5
                pieces = [sb(esE, "wp%d" % j, [128, 8, 256], BF16) for j in range(NPB)]
                wds = [sb(esE, "wd%d" % j, [128, 8, 512], BF16) for j in range(2)]
                g1 = [sb(esE, "g1_%d" % j, [128, CAP]) for j in range(2)]
                sg = [sb(esE, "sg_%d" % j, [128, CAP]) for j in range(2)]
                u1 = [sb(esE, "u1_%d" % j, [128, CAP]) for j in range(2)]
                S.do("pool", lambda g: g.memset(yacc.h[:], 0.0), [], [yacc])
                pc = 0
                ec = 0
                TG = min(8, TPC)
                for e in range(NEXP):
                    wgv = wgu_d.h[e].rearrange("(k p) n -> p k n", p=128)
                    wdv = wdn_d.h[e].rearrange("(k p) n -> p k n", p=128)
                    for tt in range(TPC):
                        S.ts("dve", Sel[:, tt, :], iota[:, 0:CAP], posm[:, tt, e:e + 1], ALU.is_equal)
                    for k in range(8):
                        pg_ = P[k % 2]
                        for tt in range(TPC):
                            S.mm(pg_[:, 0:CAP], h1bT[:, tt, k * 128:(k + 1) * 128], Sel[:, tt, :],
                                 tt == 0, tt == TPC - 1)
                        S.cp("act" if k % 2 == 0 else "dve", xeT[:, k, :], pg_[:, 0:CAP])
                    for sc in range(NSC):
                        for g0 in range(0, TPC, TG):
                            for q in range(TG):
                                S.tr(PTb[:, q * 128:(q + 1) * 128], Sel[:, g0 + q, sc * 128:(sc + 1) * 128], identb[:])
                            S.cp("act", SelT[:, sc, g0 * 128:(g0 + TG) * 128], PTb[:, 0:TG * 128])
                    for c in range(8):
                        pw = pieces[pc % NPB]
                        pc += 1
                        S.dma("pool", pw[:, :, 0:128], wgu_d.v(wgv[:, :, c * 128:(c + 1) * 128]))
                        S.dma("pool", pw[:, :, 128:256], wgu_d.v(wgv[:, :, D + c * 128:D + (c + 1) * 128]))
                        if c == 3:
                            for half in range(2):
                                S.dma("pool", wds[half][:], wdn_d.v(wdv[:, :, half * 512:(half + 1) * 512]))
                        G1, SG, U1 = g1[ec % 2], sg[ec % 2], u1[ec % 2]
                        pg, pu = P[2 + 2 * (ec % 2)], P[3 + 2 * (ec % 2)]
                        ec += 1
                        for k in range(8):
                            S.mm(pg[:, 0:CAP], pw[:, k, 0:128], xeT[:, k, :], k == 0, k == 7)
                        for k in range(8):
                            S.mm(pu[:, 0:CAP], pw[:, k, 128:256], xeT[:, k, :], k == 0, k == 7)
                        S.ts("dve", G1[:], pg[:, 0:CAP], bgu[:, e * 16 + c:e * 16 + c + 1], ALU.add, SWL, ALU.min)
                        S.act(SG[:], G1[:], AF.Sigmoid, scale=SWA)
                        S.ts("dve", U1[:], pu[:, 0:CAP], bgu1[:, e * 16 + 8 + c:e * 16 + 9 + c], ALU.add,
                             1.0 - SWL, ALU.max)
                        S.tt("dve", G1[:], G1[:], SG[:], ALU.mult)
                        S.stt("dve", actT[:, c, :], U1[:], SWL + 1.0, G1[:], ALU.min, ALU.mult)
                    for sc in range(NSC):
                        for half in range(2):
                            pb = P[half]
                            for k in range(8):
                                S.mm(pb[:, 0:512], actT[:, k, sc * 128:(sc + 1) * 128],
                                     wds[half][:, k, :], k == 0, k == 7)
                            S.cp("act", oe[:, sc, half * 512:(half + 1) * 512], pb[:, 0:512])
                    for tt in range(TPC):
                        for half in range(2):
                            pb = P[6] if half == 0 else P[1]
                            for sc in range(NSC):
                                S.mm(pb[:, 0:512], SelT[:, sc, tt * 128:(tt + 1) * 128],
                                     oe[:, sc, half * 512:(half + 1) * 512], sc == 0, sc == NSC - 1)
                            S.stt("dve", yacc[:, tt, half * 512:(half + 1) * 512], pb[:, 0:512],
                                  Gall[:, tt, e:e + 1], yacc[:, tt, half * 512:(half + 1) * 512], ALU.mult, ALU.add)
                S.barrier()
            with ExitStack() as esF:
                lnr2 = sb(esF, "lnr2", [128, 2, D])
                bdn = sb(esF, "bdn", [NEXP, D])
                S.dma("sp", lnr2[:], lngb[:, 4:6, :])
                S.dma("sp", bdn[:], bdn_d[:])
                GT = sb(esF, "GT", [NEXP, 128])
                h1r = [sb(esF, "h1r%d" % j, [128, D]) for j in range(2)]
                for tt in range(TPC):
                    rows = slice(tt * 128, (tt + 1) * 128)
                    hr = h1r[tt % 2]
                    S.dma("sp", hr[:], h1_d[rows, :])
                    S.tr(P[0][0:NEXP, 0:128], Gall[:, tt, :], ident)
                    S.cp("act", GT[:], P[0][0:NEXP, 0:128])
                    for half in range(2):
                        S.mm(P[1 + half][:, 0:512], GT[:], bdn[:, half * 512:(half + 1) * 512], True, True)
                        S.tt("dve", yacc[:, tt, half * 512:(half + 1) * 512],
                             yacc[:, tt, half * 512:(half + 1) * 512], P[1 + half][:, 0:512], ALU.add)
                    S.stt("dve", hr[:], hr[:], ALPHA, yacc[:, tt, :], ALU.mult, ALU.add)
                    layer_norm_stats(None, hr[:], "ln2", st6, mv, rs, nmr, LN_EPS)
                    S.act(hr[:], hr[:], AF.Identity, bias=nmr[:, 0:1], scale=rs[:, 0:1])
                    S.tt("dve", hr[:], hr[:], lnr2[:, 0, :], ALU.mult)
                    S.tt("pool", hr[:], hr[:], lnr2[:, 1, :], ALU.add)
                    S.dma("sp", out_d[rows, :], hr[:])
                S.wait_all("sp", [out_d])
                S.barrier()
    print("insts", S.n_inst, "waits", S.n_wait)
    return nc


def _bc(v):
    v = np.asarray(v, np.float32).reshape(1, -1)
    return np.ascontiguousarray(np.broadcast_to(v, (128, v.shape[1])))


def prep(inp, NTX):
    f = lambda k: np.asarray(inp[k], np.float32)
    x, meta = f("x"), f("meta")
    w_in = f("w_in")[0]
    R0 = 1544
    p = np.arange(128)[:, None]
    q = np.arange(128)[None, :]
    cmat = np.stack([(p == q), (p <= q), (p < q), (p > q), np.ones((128, 128), bool)], 1).astype(np.float32)
    ccol = np.zeros((128, 2), np.float32)
    ccol[112:, 0] = 1.0
    ccol[:112, 1] = NEGPAD
    lngb = np.stack([_bc(f("ln0_g")), _bc(f("ln0_b")), _bc(f("ln1_g")[0]), _bc(f("ln1_b")[0]),
                     _bc(f("ln2_g")[0]), _bc(f("ln2_b")[0])], 1)
    tile0 = np.zeros((128, D), np.float32)
    tile0[112:] = meta
    w_out = f("w_out")[0]
    bgu = f("b_gu")[0].reshape(NEXP, 16, 128).transpose(2, 0, 1).reshape(128, NEXP * 16)
    shared = {
        "lngb": np.ascontiguousarray(lngb), "cmat": np.ascontiguousarray(cmat), "ccol": ccol,
        "iota": np.ascontiguousarray(np.broadcast_to(np.arange(512, dtype=np.float32)[None, :], (128, 512))),
        "wrt": np.ascontiguousarray(f("w_router")[0]), "brt": _bc(f("b_router")[0]),
        "bgu": np.ascontiguousarray(bgu), "bdn": np.ascontiguousarray(f("b_down")[0]),
        "wgu": np.ascontiguousarray(f("w_gu")[0]), "wdn": np.ascontiguousarray(f("w_down")[0]),
    }
    perm = np.concatenate([np.concatenate([np.arange(r * 128, r * 128 + 128),
                                           np.arange(512 + r * 128, 512 + r * 128 + 128)]) for r in range(4)])
    maps = []
    for c in range(NCORES):
        b, hp = c // 4, c % 4
        cs = slice(hp * 128, hp * 128 + 128)
        hsl = np.arange(hp * 128, hp * 128 + 128)
        fcols = np.concatenate([hsl, 512 + hsl, 1024 + hsl])
        rcols = np.concatenate([R0 + hsl, R0 + 512 + hsl, R0 + 1024 + hsl, R0 + 1536 + np.arange(256)])
        mucols = rcols - R0
        rowA = np.zeros((128, NA), np.float32)

        def put(name, v):
            a, b_ = RA[name]
            rowA[:, a:b_] = _bc(v)
        put("wf0", w_in[:, 1536 + 2 * hp])
        put("wf1", w_in[:, 1536 + 2 * hp + 1])
        put("mu", f("rwkv_mu")[0][mucols])
        put("w0", f("w0")[0][cs]); put("a0", f("a0")[0][cs]); put("kk", f("k_k")[0][cs])
        put("ka", f("k_a")[0][cs]); put("rk", f("r_k")[0].reshape(-1)[cs])
        put("lg", f("lnx_g")[0][cs]); put("lb", f("lnx_b")[0][cs]); put("fg", f("fox_norm_g")[0][cs])
        put("bf", f("b_fgate")[0][2 * hp:2 * hp + 2])
        m = dict(shared)
        m["xb"] = np.ascontiguousarray(np.concatenate([tile0, x[b, :NTX * 128]], 0))
        m["rowA"] = rowA
        m["wfox"] = np.ascontiguousarray(w_in[:, fcols])
        m["wrw"] = np.ascontiguousarray(w_in[:, rcols])
        m["w2a2"] = np.ascontiguousarray(np.concatenate([f("w2")[0][:, cs], f("a2")[0][:, cs]], 0))
        m["g2"] = np.ascontiguousarray(f("g2")[0][:, cs])
        m["woutc"] = np.ascontiguousarray(np.concatenate([w_out[hp * 128:hp * 128 + 128],
                                                          w_out[512 + hp * 128:512 + hp * 128 + 128]], 0))
        TPC = NTX // 4
        m["xc"] = np.ascontiguousarray(x[b, hp * TPC * 128:(hp + 1) * TPC * 128])
        maps.append(m)
    return maps


_NC_CACHE = {}


def kernel(**inputs):
    NTX = 64
    if NTX not in _NC_CACHE:
        _NC_CACHE[NTX] = build(NTX, stage="full")
    nc = _NC_CACHE[NTX]
    maps = prep(inputs, NTX)
    res = run_bass_kernel_spmd(nc, maps, core_ids=list(range(NCORES)))
    TPC = NTX // 4
    out = np.zeros((2, NTX * 128, D), np.float32)
    for c in range(NCORES):
        b, hp = c // 4, c % 4
        out[b, hp * TPC * 128:(hp + 1) * TPC * 128] = np.asarray(res.results[c]["out"], np.float32)
    return out
```
